# Optimizing a Trainium2 kernel written in Bass

```python
import math
import jax, jax.numpy as jnp
from jax import lax
import numpy as np

D_MODEL = 1024
BATCH = 32
SEQ = 2048
DEPTH = 2

MIX_WIDTH = D_MODEL
DN_WIDTH = MIX_WIDTH // 2
DN_HEADS = 4
DN_HEAD_DIM = DN_WIDTH // DN_HEADS
CONV_WIDTH = 4
CHUNK = 64
SSM_WIDTH = MIX_WIDTH - DN_WIDTH
SSM_GROUP = 16
SSM_GROUPS = SSM_WIDTH // SSM_GROUP
SSM_STATE = 64
EPS = 1e-6
COL_QKV = 3 * DN_WIDTH
COL_ZDN = COL_QKV + DN_WIDTH
COL_BETA = COL_ZDN + DN_HEADS
COL_ALPHA = COL_BETA + DN_HEADS
COL_U = COL_ALPHA + SSM_WIDTH
IN_COLS = COL_U + SSM_WIDTH

kernel_name = "hybrid_gdn_s5_parallel_heads"


def rms_norm(x, g):
    xf = x.astype(jnp.float32)
    y = xf * lax.rsqrt(jnp.mean(xf * xf, axis=-1, keepdims=True) + EPS)
    return (y * g.astype(jnp.float32)).astype(x.dtype)


def l2_normalize(x):
    return x * lax.rsqrt(jnp.sum(x * x, axis=-1, keepdims=True) + EPS)


def causal_dwconv(x, w):
    c = x.shape[-1]
    return lax.conv_general_dilated(
        x, w[:, None, :].astype(x.dtype), window_strides=(1,),
        padding=[(CONV_WIDTH - 1, 0)],
        dimension_numbers=("NWC", "WIO", "NWC"), feature_group_count=c)


def gated_delta_rule(q, k, v, g, beta):
    bsz, s, h, dk = q.shape
    dv = v.shape[-1]
    n = s // CHUNK
    def chunks(t):
        t = t.reshape((bsz, n, CHUNK, h) + t.shape[3:])
        return jnp.moveaxis(t, 3, 1)
    q = chunks(q * (dk ** -0.5))
    k = chunks(k)
    v = chunks(v)
    gc = jnp.cumsum(chunks(g), axis=-1)
    beta = chunks(beta)
    causal = jnp.tril(jnp.ones((CHUNK, CHUNK), dtype=bool))
    strict = jnp.tril(jnp.ones((CHUNK, CHUNK), dtype=bool), -1)
    diff = gc[..., :, None] - gc[..., None, :]
    decay = jnp.where(causal, jnp.exp(jnp.where(causal, diff, 0.0)), 0.0)
    kb = k * beta[..., None]
    vb = v * beta[..., None]
    a_mat = jnp.where(strict, jnp.einsum("bhncd,bhnsd->bhncs", kb, k) * decay, 0.0)
    lower = a_mat + jnp.eye(CHUNK, dtype=a_mat.dtype)
    rhs = jnp.concatenate([vb, kb * jnp.exp(gc)[..., None]], axis=-1)
    sol = lax.linalg.triangular_solve(lower, rhs, left_side=True, lower=True, unit_diagonal=True)
    u, w = sol[..., :dv], sol[..., dv:]
    intra = jnp.where(causal, jnp.einsum("bhncd,bhnsd->bhncs", q, k) * decay, 0.0)

    def step(state, inp):
        q_i, k_i, u_i, w_i, g_i, a_i = inp
        v_new = u_i - jnp.einsum("bhcd,bhdv->bhcv", w_i, state)
        o = (jnp.einsum("bhcd,bhdv->bhcv", q_i * jnp.exp(g_i)[..., None], state)
             + jnp.einsum("bhcs,bhsv->bhcv", a_i, v_new))
        g_last = g_i[..., -1]
        k_dec = k_i * jnp.exp(g_last[..., None] - g_i)[..., None]
        state = state * jnp.exp(g_last)[..., None, None] + jnp.einsum("bhcd,bhcv->bhdv", k_dec, v_new)
        return state, o

    xs = tuple(jnp.moveaxis(t, 2, 0) for t in (q, k, u, w, gc, intra))
    state0 = jnp.zeros((bsz, h, dk, dv), jnp.float32)
    _, o = lax.scan(step, state0, xs)
    o = jnp.transpose(o, (1, 0, 3, 2, 4))
    return o.reshape(bsz, s, h, dv)


def s5_scan(u, a_re, a_im, b_re, b_im, c_re, c_im, d, log_dt):
    bsz, s, _ = u.shape
    ug = u.reshape(bsz, s, SSM_GROUPS, SSM_GROUP)
    dt = jnp.exp(log_dt.astype(jnp.float32))[:, None]
    a_re = a_re.astype(jnp.float32)
    a_im = a_im.astype(jnp.float32)
    mag = jnp.exp(a_re * dt)
    ang = a_im * dt
    lb_re, lb_im = mag * jnp.cos(ang), mag * jnp.sin(ang)
    den = a_re * a_re + a_im * a_im
    num_re, num_im = lb_re - 1.0, lb_im
    coef_re = (num_re * a_re + num_im * a_im) / den
    coef_im = (num_im * a_re - num_re * a_im) / den
    b_re = b_re.astype(jnp.float32)
    b_im = b_im.astype(jnp.float32)
    bb_re = coef_re[..., None] * b_re - coef_im[..., None] * b_im
    bb_im = coef_re[..., None] * b_im + coef_im[..., None] * b_re
    bu_re = jnp.einsum("bsgh,gph->sbgp", ug, bb_re)
    bu_im = jnp.einsum("bsgh,gph->sbgp", ug, bb_im)
    lam_re = jnp.broadcast_to(lb_re[None, None], (s, 1, SSM_GROUPS, SSM_STATE))
    lam_im = jnp.broadcast_to(lb_im[None, None], (s, 1, SSM_GROUPS, SSM_STATE))

    def combine(e1, e2):
        a1r, a1i, b1r, b1i = e1
        a2r, a2i, b2r, b2i = e2
        return (a2r * a1r - a2i * a1i, a2r * a1i + a2i * a1r,
                a2r * b1r - a2i * b1i + b2r, a2r * b1i + a2i * b1r + b2i)

    _, _, st_re, st_im = lax.associative_scan(combine, (lam_re, lam_im, bu_re, bu_im), axis=0)
    y = (jnp.einsum("sbgp,ghp->bsgh", st_re, c_re.astype(jnp.float32))
         - jnp.einsum("sbgp,ghp->bsgh", st_im, c_im.astype(jnp.float32)))
    return y.reshape(bsz, s, SSM_WIDTH) + d.astype(jnp.float32) * u


def hybrid_layer(x, pre_g, post_g, w_in, conv_w, dn_a_log, dn_dt_bias, dn_norm_g,
                 ssm_a_re, ssm_a_im, ssm_b_re, ssm_b_im, ssm_c_re, ssm_c_im, ssm_d,
                 ssm_log_dt, glu_w, glu_b, w_out):
    bsz, s, _ = x.shape
    h = rms_norm(x, pre_g)
    p = h @ w_in
    qkv = p[..., :COL_QKV]
    z_dn = p[..., COL_QKV:COL_ZDN]
    beta_logit = p[..., COL_ZDN:COL_BETA]
    alpha = p[..., COL_BETA:COL_ALPHA]
    u_ssm = p[..., COL_ALPHA:COL_U]
    z_ssm = p[..., COL_U:]

    qkv = jax.nn.silu(causal_dwconv(qkv, conv_w)).astype(jnp.float32)
    q = l2_normalize(qkv[..., :DN_WIDTH].reshape(bsz, s, DN_HEADS, DN_HEAD_DIM))
    k = l2_normalize(qkv[..., DN_WIDTH:2 * DN_WIDTH].reshape(bsz, s, DN_HEADS, DN_HEAD_DIM))
    v = qkv[..., 2 * DN_WIDTH:].reshape(bsz, s, DN_HEADS, DN_HEAD_DIM)
    beta = jax.nn.sigmoid(beta_logit.astype(jnp.float32))
    g = -jnp.exp(dn_a_log.astype(jnp.float32)) * jax.nn.softplus(
        alpha.astype(jnp.float32) + dn_dt_bias.astype(jnp.float32))
    o = gated_delta_rule(q, k, v, g, beta)
    o = rms_norm(o, dn_norm_g) * jax.nn.silu(
        z_dn.astype(jnp.float32).reshape(bsz, s, DN_HEADS, DN_HEAD_DIM))
    o_dn = o.reshape(bsz, s, DN_WIDTH).astype(x.dtype)

    y = s5_scan(u_ssm.astype(jnp.float32), ssm_a_re, ssm_a_im, ssm_b_re, ssm_b_im,
                ssm_c_re, ssm_c_im, ssm_d, ssm_log_dt)
    y = jax.nn.gelu(y)
    gl = y @ glu_w.astype(jnp.float32) + glu_b.astype(jnp.float32)
    y = gl[..., :SSM_WIDTH] * jax.nn.sigmoid(gl[..., SSM_WIDTH:])
    o_ssm = (y * jax.nn.silu(z_ssm.astype(jnp.float32))).astype(x.dtype)

    mix = jnp.concatenate([o_dn, o_ssm], axis=-1) @ w_out
    return x + rms_norm(mix, post_g)


def setup_inputs(seed: int = 0) -> dict:
    key = jax.random.key(seed)
    ks = jax.random.split(key, 20)
    f32 = jnp.float32
    nrm = lambda k, shp, sc: sc * jax.random.normal(k, shp, f32)
    log_lo, log_hi = math.log(1e-3), math.log(1e-1)
    dn_dt = jnp.exp(jax.random.uniform(ks[6], (DEPTH, DN_HEADS), f32, log_lo, log_hi))
    return {
        "x": jax.random.normal(ks[0], (BATCH, SEQ, D_MODEL), f32),
        "pre_norm_g": 1.0 + nrm(ks[1], (DEPTH, D_MODEL), 0.05),
        "post_norm_g": 1.0 + nrm(ks[2], (DEPTH, D_MODEL), 0.05),
        "w_in": nrm(ks[3], (DEPTH, D_MODEL, IN_COLS), D_MODEL ** -0.5),
        "conv_w": nrm(ks[4], (DEPTH, CONV_WIDTH, COL_QKV), CONV_WIDTH ** -0.5),
        "dn_a_log": jnp.log(jax.random.uniform(ks[5], (DEPTH, DN_HEADS), f32, 1.0, 16.0)),
        "dn_dt_bias": dn_dt + jnp.log(-jnp.expm1(-dn_dt)),
        "dn_norm_g": 1.0 + nrm(ks[7], (DEPTH, DN_HEAD_DIM), 0.05),
        "ssm_a_re": -0.5 + nrm(ks[8], (DEPTH, SSM_GROUPS, SSM_STATE), 0.01),
        "ssm_a_im": math.pi * jnp.arange(SSM_STATE, dtype=f32) + nrm(ks[9], (DEPTH, SSM_GROUPS, SSM_STATE), 0.01),
        "ssm_b_re": nrm(ks[10], (DEPTH, SSM_GROUPS, SSM_STATE, SSM_GROUP), (2 * SSM_GROUP) ** -0.5),
        "ssm_b_im": nrm(ks[11], (DEPTH, SSM_GROUPS, SSM_STATE, SSM_GROUP), (2 * SSM_GROUP) ** -0.5),
        "ssm_c_re": nrm(ks[12], (DEPTH, SSM_GROUPS, SSM_GROUP, SSM_STATE), SSM_STATE ** -0.5),
        "ssm_c_im": nrm(ks[13], (DEPTH, SSM_GROUPS, SSM_GROUP, SSM_STATE), SSM_STATE ** -0.5),
        "ssm_d": nrm(ks[14], (DEPTH, SSM_WIDTH), 1.0),
        "ssm_log_dt": jax.random.uniform(ks[15], (DEPTH, SSM_GROUPS), f32, log_lo, log_hi),
        "glu_w": nrm(ks[16], (DEPTH, SSM_WIDTH, 2 * SSM_WIDTH), SSM_WIDTH ** -0.5),
        "glu_b": nrm(ks[17], (DEPTH, 2 * SSM_WIDTH), 0.01),
        "w_out": nrm(ks[18], (DEPTH, MIX_WIDTH, D_MODEL), MIX_WIDTH ** -0.5),
    }


def reference(x, pre_norm_g, post_norm_g, w_in, conv_w, dn_a_log, dn_dt_bias, dn_norm_g,
              ssm_a_re, ssm_a_im, ssm_b_re, ssm_b_im, ssm_c_re, ssm_c_im, ssm_d,
              ssm_log_dt, glu_w, glu_b, w_out):
    for l in range(DEPTH):
        x = hybrid_layer(x, pre_norm_g[l], post_norm_g[l], w_in[l], conv_w[l], dn_a_log[l],
                         dn_dt_bias[l], dn_norm_g[l], ssm_a_re[l], ssm_a_im[l], ssm_b_re[l],
                         ssm_b_im[l], ssm_c_re[l], ssm_c_im[l], ssm_d[l], ssm_log_dt[l],
                         glu_w[l], glu_b[l], w_out[l])
    return x
```

```python
import numpy as np
from contextlib import ExitStack
import ml_dtypes
import concourse.bass as bass
import concourse.mybir as mybir
from concourse.bass_utils import run_bass_kernel_spmd

F32 = mybir.dt.float32
BF16 = mybir.dt.bfloat16
AF = mybir.ActivationFunctionType
ALU = mybir.AluOpType

D = 1024
INC = 3080
NH = 4
EPS = 1e-6
T = 128
NCORES = 8


class Buf:
    def __init__(self, name, t, blk, nblk, psum=False):
        self.name, self.t, self.blk, self.nblk, self.psum = name, t, blk, nblk, psum

    def r(self, lo=0, hi=None):
        if hi is None:
            hi = self.blk * self.nblk
        return (self, lo, hi)


class Sub:
    def __init__(self, parent, off, cols):
        self.parent, self.off, self.cols = parent, off, cols
        self.t = parent.t[:, off:off + cols]
        self.name = parent.name

    def r(self, lo=0, hi=None):
        if hi is None:
            hi = self.cols
        return (self.parent, self.off + lo, self.off + hi)


class Sched:
    ENG = ("pe", "act", "dve", "pool", "sp")

    def __init__(self, sems):
        self.sems = sems
        self.cnt = {}
        self.streams = {e: [] for e in self.ENG}
        self.lastw = {}
        self.readers = {}
        self.seen = {e: {} for e in self.ENG}
        self.free_dma = sorted([k for k in sems if k.startswith("dma")], key=lambda s: int(s[3:]))
        self.bufsem = {}
        self.pending = {e: False for e in self.ENG}
        self.group = None
        self.label = None

    def _blocks(self, reg):
        b, lo, hi = reg
        return [(b.name, i) for i in range(lo // b.blk, (hi - 1) // b.blk + 1)]

    def _need(self, eng, waits, dep):
        if dep is None:
            return
        sem, val, deng, big = dep
        if deng == eng and not sem.startswith("dma") and (eng == "pe" or big):
            return
        if self.seen[eng].get(sem, 0) >= val:
            return
        waits[sem] = max(waits.get(sem, 0), val)

    def op(self, eng, fn, reads=(), writes=(), dma_sem=None, sig=True):
        reads = list(reads)
        writes = list(writes)
        writes += [r for r in reads if r[0].psum]
        reads = [r for r in reads if not r[0].psum]
        waits = {}
        for reg in reads:
            for k in self._blocks(reg):
                self._need(eng, waits, self.lastw.get(k))
        for reg in writes:
            for k in self._blocks(reg):
                self._need(eng, waits, self.lastw.get(k))
                for sem, (val, deng, rbig) in self.readers.get(k, {}).items():
                    self._need(eng, waits, (sem, val, deng, rbig))
        for sem, val in waits.items():
            self.seen[eng][sem] = max(self.seen[eng].get(sem, 0), val)
        if dma_sem is not None:
            sem, inc = dma_sem, 16
        else:
            sem, inc = "e_" + eng, 1
        if sig:
            val = self.cnt.get(sem, 0) + inc
            self.cnt[sem] = val
            if dma_sem is None:
                self.pending[eng] = False
        else:
            val = self.cnt.get(sem, 0) + inc
            self.pending[eng] = True
            inc = 0
        big = getattr(fn, "n", 0) >= BIG_N and eng != "pool"
        me = (sem, val, eng, big)
        for reg in reads:
            for k in self._blocks(reg):
                self.readers.setdefault(k, {})[sem] = (val, eng, big)
        for reg in writes:
            for k in self._blocks(reg):
                self.lastw[k] = me
                self.readers[k] = {}
                if self.group is not None and dma_sem is not None:
                    self.group.append(k)
        self.streams[eng].append((list(waits.items()), fn, sem, inc, self.label))
        return me

    def group_begin(self):
        self.group = []

    def group_end(self, sem, eng="sp"):
        fin = (sem, self.cnt.get(sem, 0), eng, False)
        for k in self.group:
            self.lastw[k] = fin
        self.group = None

    def dsem(self, key):
        if key not in self.bufsem:
            self.bufsem[key] = self.free_dma.pop(0)
        return self.bufsem[key]

    def final_wait(self, eng, deps):
        waits = {}
        for d in deps:
            self._need(eng, waits, d)
        self.streams[eng].append((list(waits.items()), None, None, 0, None))

    def emit(self, block):
        sems = self.sems
        for e in self.ENG:
            assert not self.pending[e], "unsignalled tail on " + e

        def play(engobj, name):
            for waits, fn, sem, inc, label in self.streams[name]:
                for s, v in waits:
                    engobj.wait_ge(sems[s], v)
                if fn is not None:
                    ins = fn(engobj)
                    if inc:
                        ins.then_inc(sems[sem], inc)
                    if label is not None and ANNOTATE:
                        ins.annotate(label)

        @block.tensor
        def _(e):
            play(e, "pe")

        @block.scalar
        def _(e):
            play(e, "act")

        @block.vector
        def _(e):
            play(e, "dve")

        @block.gpsimd
        def _(e):
            play(e, "pool")

        @block.sync
        def _(e):
            play(e, "sp")


BIG_N = 10 ** 9


def _sz(fn, out):
    try:
        fn.n = int(out.free_size())
    except Exception:
        fn.n = 0
    return fn


def mm(out, lhsT, rhs, start=True, stop=True):
    return lambda e: e.matmul(out, lhsT=lhsT, rhs=rhs, start=start, stop=stop)


def tr(out, in_, ident):
    return lambda e: e.transpose(out=out, in_=in_, identity=ident)


def af(out, in_, func, bias=None, scale=None, accum=None):
    kw = {}
    if bias is not None:
        kw["bias"] = bias
    if scale is not None:
        kw["scale"] = scale
    if accum is not None:
        kw["accum_out"] = accum
    return _sz(lambda e: e.activation(out=out, in_=in_, func=func, **kw), out)


def tt(out, a, b, op):
    return _sz(lambda e: e.tensor_tensor(out=out, in0=a, in1=b, op=op), out)


def ts(out, a, s1, op0, s2=None, op1=None):
    if op1 is None:
        return _sz(lambda e: e.tensor_scalar(out=out, in0=a, scalar1=s1, scalar2=None, op0=op0), out)
    return _sz(lambda e: e.tensor_scalar(out=out, in0=a, scalar1=s1, scalar2=s2, op0=op0, op1=op1), out)


def stt(out, a, sc, b, op0, op1):
    return _sz(lambda e: e.scalar_tensor_tensor(out=out, in0=a, scalar=sc, in1=b, op0=op0, op1=op1), out)


def cp(out, in_):
    return _sz(lambda e: e.tensor_copy(out=out, in_=in_), out)


def scan(out, d0, d1, init):
    return lambda e: e.tensor_tensor_scan(out=out, data0=d0, data1=d1, initial=init, op0=ALU.mult, op1=ALU.add)


def dma(out, in_):
    return lambda e: e.dma_start(out=out, in_=in_)


def mset(ap, v):
    return lambda e: e.memset(ap, v)


def build_program(nseq, slen, nlayers, debug=None):
    debug = debug or {}
    ntile = slen // T
    ntok = nseq * slen
    nc = bass.Bass("TRN2", target_bir_lowering=False)
    es = ExitStack()
    dbg_out = {}

    def din(name, shape, dt=F32):
        return nc.dram_tensor(name, list(shape), dt, kind="ExternalInput").ap()

    x_d = din("x", [ntok, D])
    out_d = nc.dram_tensor("out", [ntok, D], F32, kind="ExternalOutput").ap()
    x1_t = nc.dram_tensor("x1", [ntok, D], F32, kind="Internal").ap() if nlayers > 1 else None
    L = nlayers
    w_in_d = din("w_in", [L, D, INC])
    w_out_d = din("w_out", [L, D, D])
    glu_w_d = din("glu_w", [L, 512, 1024])
    preg_d = din("preg", [L, 128, 8])
    postg_d = din("postg", [L, 128, 1024])
    convw_d = din("convw", [L, 128, 48])
    alog_d = din("alog", [L, 128, 4])
    dtb_d = din("dtb", [L, 128, 4])
    normg_d = din("normg", [L, 128, 128])
    glub_d = din("glub", [L, 128, 8])
    ssmd_d = din("ssmd", [L, 128, 4])
    are_d = din("are", [L, 128, 16])
    aim_d = din("aim", [L, 128, 16])
    ldt_d = din("ldt", [L, 128, 16])
    bre_d = din("bre", [L, 128, 256])
    bim_d = din("bim", [L, 128, 256])
    cre_d = din("cre", [L, 128, 256])
    cim_d = din("cim", [L, 128, 256])
    identf_d = din("identf", [128, 128])
    tri_d = din("tri", [128, 128])
    blk_d = din("blkm", [128, 128])
    maska_d = din("maska", [128, 128])
    maski_d = din("maski", [128, 128])

    with es:
        def sb(name, cols, dt=F32, blk=None):
            t = es.enter_context(nc.sbuf_tensor("sb_" + name, [128, cols], dt))
            blk = blk or cols
            return Buf(name, t, blk, cols // blk)

        def ps(name, cols, dt):
            t = es.enter_context(nc.psum_tensor("ps_" + name, [128, cols], dt))
            return Buf(name, t, cols, 1, psum=True)

        class DBuf:
            def __init__(self, name, n):
                self.name, self.blk, self.nblk, self.psum = name, 1, n, False

        x1_b = DBuf("x1d", nseq * ntile)

        identf = sb("identf", 128)
        identb = sb("identb", 128, BF16)
        onesf = sb("onesf", 128)
        onesb = sb("onesb", 128, BF16)
        tri = sb("tri", 128)
        blkm = sb("blkm", 128)
        maska = sb("maska", 128)
        maski = sb("maski", 128)
        win_b = sb("win_b", 8 * INC, BF16)
        wout_b = sb("wout_b", 8 * 1024, BF16)
        glu_b16 = sb("glu_b16", 4 * 1024, BF16)
        SCH = 770
        stage = [sb("stage%d" % i, 1024) for i in range(2)]
        preg = sb("preg", 8)
        gpost = sb("gpost", 1024)
        convw = sb("convw", 48)
        cdiag = sb("cdiag", 48 * 128, BF16)
        alog = sb("alog", 4)
        ealog = sb("ealog", 4)
        dtb = sb("dtb", 4)
        normg = sb("normg", 128)
        glub = sb("glub", 8)
        ssmd = sb("ssmd", 4)
        are = sb("are", 16)
        aim = sb("aim", 16)
        ldt = sb("ldt", 16)
        sm = sb("sm", 16 * 24, blk=16)
        rmag = sb("rmag", 16)
        zpad = sb("zpad", 16 * 128, BF16)
        bpad = sb("bpad", 32 * 128, BF16, blk=128)
        cpad = sb("cpad", 32 * 128, BF16)
        tabc = sb("tabc", 16 * 128)
        tabs = sb("tabs", 16 * 128)
        xt = sb("xt", 1024)
        st8 = sb("st8", 32, blk=1)
        xb = sb("xb", 1024, BF16)
        hT = sb("hT", 1024, BF16)
        pT = sb("pT", 12 * 131, BF16)
        uT = sb("uT", 512, BF16)
        zs = sb("zs", 512, BF16)
        gate = sb("gate", 512, BF16)
        qks = sb("qks", 1024, blk=512)
        vT = sb("vT", 512, BF16)
        sq = sb("sq", 1024, BF16)
        tmpbig = sb("tmpbig", 2048, blk=512)
        rinv = Sub(tmpbig, 0, 1024)
        qkn = sb("qkn", 1024, BF16)
        gts = sb("gts", 64, blk=4)
        gh = Sub(tmpbig, 1024, 512)
        gcrow = sb("gcrow", 512)
        egrow = sb("egrow", 512)
        qgT = sb("qgT", 512, BF16)
        kbg = sb("kbg", 512, BF16)
        kdec = sb("kdec", 512, BF16)
        vb = sb("vb", 512, BF16)
        gdf = sb("gdf", 16 * 128, F32, blk=128)
        gdb = sb("gdb", 16 * 128, BF16, blk=128)
        gd = [dict(
            x=Sub(gdf, (4 * p + 0) * 128, 128), xT=Sub(gdf, (4 * p + 1) * 128, 128), pt=Sub(gdf, (4 * p + 2) * 128, 128),
            usb=Sub(gdf, (4 * p + 3) * 128, 128),
            intra=Sub(gdb, (4 * p + 0) * 128, 128), ttb=Sub(gdb, (4 * p + 1) * 128, 128),
            wtb=Sub(gdb, (4 * p + 2) * 128, 128), vn=Sub(gdb, (4 * p + 3) * 128, 128)) for p in range(4)]
        y_sb = Sub(gdf, 0, 512)
        sg = Sub(gdf, 512, 512)
        xo = Sub(gdf, 1024, 1024)
        gy = Sub(gdb, 0, 512)
        junk = Sub(gdb, 1024, 1024)
        s_f = sb("s_f", 512, blk=128)
        s_b = sb("s_b", 512, BF16, blk=128)
        o_sb = sb("o_sb", 512, blk=128)
        mixT = sb("mixT", 1024, BF16, blk=128)
        tmp = [Sub(tmpbig, 512 * i, 512) for i in range(4)]
        sre_b = sb("sre_b", 512, BF16)
        sim_b = sb("sim_b", 512, BF16)
        odn = sre_b
        s5st = sb("s5st", 32, blk=16)
        xt_ = [xt, Sub(stage[0], 0, 1024)]
        uT_ = [uT, sb("uT2", 512, BF16)]
        zs_ = [zs, sb("zs2", 512, BF16)]
        gate_ = [gate, sb("gate2", 512, BF16)]
        lg_ = [sb("lg0", 8), sb("lg1", 8)]

        B = [ps("B%d" % i, 512, F32) for i in range(7)]
        BT = ps("BT", 1024, BF16)

        names = ["e_pe", "e_act", "e_dve", "e_pool", "e_sp"] + ["dma%d" % i for i in range(24)]
        sems = {n: es.enter_context(nc.semaphore(n)) for n in names}
        block = es.enter_context(nc.Block())
        S = Sched(sems)
        S.tilecount = 0
        pe = lambda fn, r=(), w=(), sig=True: S.op("pe", fn, r, w, sig=sig)
        act = lambda fn, r=(), w=(): S.op("act", fn, r, w)
        dve = lambda fn, r=(), w=(): S.op("dve", fn, r, w)
        pool = lambda fn, r=(), w=(): S.op("pool", fn, r, w)

        def load(buf_regs, out_ap, in_ap, key, reads=()):
            return S.op("sp", dma(out_ap, in_ap), reads, buf_regs, dma_sem=S.dsem(key))

        def tap(name, ap, cols, dt=F32, reads=()):
            d = nc.dram_tensor("dbg_" + name, [128, cols], dt, kind="ExternalOutput").ap()
            dbg_out[name] = S.op("sp", dma(d[:, :], ap), reads, (), dma_sem=S.dsem("dbg_" + name))

        S.group_begin()
        for buf, src in ((identf, identf_d), (tri, tri_d), (blkm, blk_d), (maska, maska_d), (maski, maski_d)):
            load([buf.r()], buf.t[:], src[:, :], "const")
        S.group_end(S.dsem("const"))
        act(af(identb.t[:], identf.t[:], AF.Copy), [identf.r()], [identb.r()])
        dve(mset(onesf.t[:], 1.0), (), [onesf.r()])
        dve(mset(onesb.t[:], 1.0), (), [onesb.r()])

        def sm_(i):
            return sm.t[:, 16 * i:16 * i + 16], sm.r(16 * i, 16 * i + 16)

        def layer_setup(l):
            S.group_begin()
            key = "par"
            for buf, src in ((preg, preg_d), (gpost, postg_d), (convw, convw_d), (alog, alog_d), (dtb, dtb_d),
                             (normg, normg_d), (glub, glub_d), (ssmd, ssmd_d), (are, are_d), (aim, aim_d), (ldt, ldt_d)):
                load([buf.r()], buf.t[:], src[l], key)
            load([tmp[0].r()], tmp[0].t[:, 0:256], bre_d[l], key)
            load([tmp[1].r()], tmp[1].t[:, 0:256], bim_d[l], key)
            load([tmp[2].r()], tmp[2].t[:, 0:256], cre_d[l], key)
            load([tmp[3].r()], tmp[3].t[:, 0:256], cim_d[l], key)
            S.group_end(S.dsem(key))

            n = 0
            for kc in range(8):
                for c0 in range(0, INC, SCH):
                    stg = stage[n % 2]
                    n += 1
                    load([stg.r()], stg.t[:, 0:SCH], w_in_d[l, kc * 128:(kc + 1) * 128, c0:c0 + SCH], stg.name)
                    act(af(win_b.t[:, kc * INC + c0: kc * INC + c0 + SCH], stg.t[:, 0:SCH], AF.Copy, scale=preg.t[:, kc:kc + 1]),
                        [stg.r(), preg.r()], [win_b.r()])
            for kc in range(8):
                stg = stage[n % 2]
                n += 1
                load([stg.r()], stg.t[:, 0:1024], w_out_d[l, kc * 128:(kc + 1) * 128, :], stg.name)
                act(af(wout_b.t[:, kc * 1024:(kc + 1) * 1024], stg.t[:, 0:1024], AF.Copy), [stg.r()], [wout_b.r()])
            for kc in range(4):
                stg = stage[n % 2]
                n += 1
                load([stg.r()], stg.t[:, 0:1024], glu_w_d[l, kc * 128:(kc + 1) * 128, :], stg.name)
                act(af(glu_b16.t[:, kc * 1024:(kc + 1) * 1024], stg.t[:, 0:1024], AF.Copy), [stg.r()], [glu_b16.r()])
            for j in range(12):
                for k in range(4):
                    i = j * 4 + k
                    pool(ts(cdiag.t[:, i * 128:(i + 1) * 128], identf.t[:], convw.t[:, i:i + 1], ALU.mult),
                         [identf.r(), convw.r()], [cdiag.r()])
            act(af(ealog.t[:], alog.t[:], AF.Exp), [alog.r()], [ealog.r()])

            (dt_, dt_r), (t0, t0r), (ang, angr), (c_, c_r), (s_, s_r) = sm_(0), sm_(1), sm_(2), sm_(3), sm_(4)
            (c2, c2r), (s2, s2r), (cs, csr) = sm_(5), sm_(6), sm_(7)
            (lbr, lbrr), (lbi, lbir), (den, denr), (cr, crr), (ci, cir) = sm_(8), sm_(9), sm_(10), sm_(11), sm_(12)
            (u1, u1r), (u2, u2r) = sm_(13), sm_(14)
            act(af(dt_, ldt.t[:], AF.Exp), [ldt.r()], [dt_r])
            dve(tt(t0, are.t[:], dt_, ALU.mult), [are.r(), dt_r], [t0r])
            act(af(rmag.t[:], t0, AF.Exp), [t0r], [rmag.r()])
            dve(tt(ang, aim.t[:], dt_, ALU.mult), [aim.r(), dt_r], [angr])
            act(af(s_, ang, AF.Sin, scale=1.0 / 16), [angr], [s_r])
            act(af(c_, ang, AF.Sin, scale=1.0 / 16, bias=float(np.pi / 2)), [angr], [c_r])
            for _ in range(4):
                dve(tt(c2, c_, c_, ALU.mult), [c_r], [c2r])
                dve(tt(s2, s_, s_, ALU.mult), [s_r], [s2r])
                dve(tt(cs, c_, s_, ALU.mult), [c_r, s_r], [csr])
                dve(tt(c_, c2, s2, ALU.subtract), [c2r, s2r], [c_r])
                dve(ts(s_, cs, 2.0, ALU.mult), [csr], [s_r])
            dve(tt(lbr, rmag.t[:], c_, ALU.mult), [rmag.r(), c_r], [lbrr])
            dve(tt(lbi, rmag.t[:], s_, ALU.mult), [rmag.r(), s_r], [lbir])
            dve(tt(u1, are.t[:], are.t[:], ALU.mult), [are.r()], [u1r])
            dve(tt(u2, aim.t[:], aim.t[:], ALU.mult), [aim.r()], [u2r])
            dve(tt(den, u1, u2, ALU.add), [u1r, u2r], [denr])
            dve(lambda e: e.reciprocal(out=den, in_=den), [denr], [denr])
            dve(ts(u1, lbr, -1.0, ALU.add), [lbrr], [u1r])
            dve(tt(cr, u1, are.t[:], ALU.mult), [u1r, are.r()], [crr])
            dve(tt(u2, lbi, aim.t[:], ALU.mult), [lbir, aim.r()], [u2r])
            dve(tt(cr, cr, u2, ALU.add), [crr, u2r], [crr])
            dve(tt(cr, cr, den, ALU.mult), [crr, denr], [crr])
            dve(tt(ci, lbi, are.t[:], ALU.mult), [lbir, are.r()], [cir])
            dve(tt(u2, u1, aim.t[:], ALU.mult), [u1r, aim.r()], [u2r])
            dve(tt(ci, ci, u2, ALU.subtract), [cir, u2r], [cir])
            dve(tt(ci, ci, den, ALU.mult), [cir, denr], [cir])
            b_re3 = tmp[0].t[:, 0:256].rearrange("p (a h) -> p a h", h=16)
            b_im3 = tmp[1].t[:, 0:256].rearrange("p (a h) -> p a h", h=16)
            w1 = tmp[0].t[:, 256:512].rearrange("p (a h) -> p a h", h=16)
            w2 = tmp[1].t[:, 256:512].rearrange("p (a h) -> p a h", h=16)
            crb = cr.unsqueeze(2).to_broadcast([128, 16, 16])
            cib = ci.unsqueeze(2).to_broadcast([128, 16, 16])
            dve(tt(w1, b_re3, crb, ALU.mult), [tmp[0].r(), crr], [tmp[0].r()])
            dve(tt(w2, b_im3, cib, ALU.mult), [tmp[1].r(), cir], [tmp[1].r()])
            dve(tt(w1, w1, w2, ALU.subtract), [tmp[0].r(), tmp[1].r()], [tmp[0].r()])
            dve(tt(w2, b_im3, crb, ALU.mult), [tmp[1].r(), crr], [tmp[1].r()])
            dve(tt(b_re3, b_re3, cib, ALU.mult), [tmp[0].r(), cir], [tmp[0].r()])
            dve(tt(w2, w2, b_re3, ALU.add), [tmp[0].r(), tmp[1].r()], [tmp[1].r()])
            for comp, src in ((0, tmp[0]), (1, tmp[1])):
                dve(mset(zpad.t[:], 0.0), (), [zpad.r()])
                src3 = src.t[:, 256:512].rearrange("p (a h) -> p a h", h=16)
                z3 = zpad.t[:].rearrange("p (a c) -> p a c", c=128)
                for g2 in range(2):
                    for pm in range(4):
                        c0 = 32 * pm + 16 * g2
                        dve(cp(z3[64 * g2:64 * g2 + 64, pm::4, c0:c0 + 16], src3[64 * g2:64 * g2 + 64, pm::4, :]),
                            [src.r()], [zpad.r()])
                for rnd in range(2):
                    for i in range(8):
                        pair = rnd * 8 + i
                        pe(tr(BT.t[:, i * 128:(i + 1) * 128], zpad.t[:, pair * 128:(pair + 1) * 128], identb.t[:]),
                           [zpad.r(), identb.r()], [BT.r()], sig=(i == 7))
                    for i in range(8):
                        pair = rnd * 8 + i
                        k = pair * 2 + comp
                        act(af(bpad.t[:, k * 128:(k + 1) * 128], BT.t[:, i * 128:(i + 1) * 128], AF.Copy), [BT.r()],
                            [bpad.r(k * 128, (k + 1) * 128)])
            dve(mset(cpad.t[:], 0.0), (), [cpad.r()])
            cp3 = cpad.t[:].rearrange("p (a c) -> p a c", c=128)
            for comp, src in ((0, tmp[2]), (1, tmp[3])):
                src3 = src.t[:, 0:256].rearrange("p (a h) -> p a h", h=16)
                for g2 in range(2):
                    for pm in range(4):
                        c0 = 32 * pm + 16 * g2
                        dst = cp3[64 * g2:64 * g2 + 64, (2 * pm + comp)::8, c0:c0 + 16]
                        s_ap = src3[64 * g2:64 * g2 + 64, pm::4, :]
                        if comp == 0:
                            dve(cp(dst, s_ap), [src.r()], [cpad.r()])
                        else:
                            dve(ts(dst, s_ap, -1.0, ALU.mult), [src.r()], [cpad.r()])
            tc3 = tabc.t[:].rearrange("p (a t) -> p a t", t=128)
            ts3 = tabs.t[:].rearrange("p (a t) -> p a t", t=128)
            w3 = [tmp[i].t[:, 0:1024 // 2].rearrange("p (a t) -> p a t", a=16) for i in range(4)]
            dve(cp(tc3[:, :, 0:1], c_.unsqueeze(2)), [c_r], [tabc.r()])
            dve(cp(ts3[:, :, 0:1], s_.unsqueeze(2)), [s_r], [tabs.r()])
            n_ = 1
            while n_ < 128:
                cn = tc3[:, :, n_ - 1:n_].to_broadcast([128, 16, n_])
                sn = ts3[:, :, n_ - 1:n_].to_broadcast([128, 16, n_])
                if n_ <= 32:
                    a1, a2, a3, a4 = (w3[i][:, :, 0:n_] for i in range(4))
                    regs = [tmp[i].r() for i in range(4)]
                    dve(tt(a1, tc3[:, :, 0:n_], cn, ALU.mult), [tabc.r()], [regs[0]])
                    dve(tt(a2, ts3[:, :, 0:n_], sn, ALU.mult), [tabs.r()], [regs[1]])
                    dve(tt(a3, tc3[:, :, 0:n_], sn, ALU.mult), [tabc.r(), tabs.r()], [regs[2]])
                    dve(tt(a4, ts3[:, :, 0:n_], cn, ALU.mult), [tabc.r(), tabs.r()], [regs[3]])
                    dve(tt(tc3[:, :, n_:2 * n_], a1, a2, ALU.subtract), [regs[0], regs[1]], [tabc.r()])
                    dve(tt(ts3[:, :, n_:2 * n_], a3, a4, ALU.add), [regs[2], regs[3]], [tabs.r()])
                else:
                    for h0 in (0, 32):
                        cnh = tc3[:, :, n_ - 1:n_].to_broadcast([128, 16, 32])
                        snh = ts3[:, :, n_ - 1:n_].to_broadcast([128, 16, 32])
                        a1, a2, a3, a4 = (w3[i][:, :, 0:32] for i in range(4))
                        regs = [tmp[i].r() for i in range(4)]
                        dve(tt(a1, tc3[:, :, h0:h0 + 32], cnh, ALU.mult), [tabc.r()], [regs[0]])
                        dve(tt(a2, ts3[:, :, h0:h0 + 32], snh, ALU.mult), [tabs.r()], [regs[1]])
                        dve(tt(a3, tc3[:, :, h0:h0 + 32], snh, ALU.mult), [tabc.r(), tabs.r()], [regs[2]])
                        dve(tt(a4, ts3[:, :, h0:h0 + 32], cnh, ALU.mult), [tabc.r(), tabs.r()], [regs[3]])
                        dve(tt(tc3[:, :, n_ + h0:n_ + h0 + 32], a1, a2, ALU.subtract), [regs[0], regs[1]], [tabc.r()])
                        dve(tt(ts3[:, :, n_ + h0:n_ + h0 + 32], a3, a4, ALU.add), [regs[2], regs[3]], [tabs.r()])
                n_ *= 2

        ST_SSQ, ST_RSTD, ST_SSQ2A, ST_SSQ2B, ST_RSTD2, ST_OSSQ, ST_ORSTD = 0, 1, 2, 3, 4, 8, 12

        def stc(i, n=1):
            return st8.t[:, i:i + n], st8.r(i, i + n)

        G_BETA, G_X, G_NA, G_E, G_L, G_SP, G_G, G_GC, G_GL, G_BG, G_KD, G_T = range(12)

        def gt(i):
            return gts.t[:, 4 * i:4 * i + 4], gts.r(4 * i, 4 * i + 4)

        def front_a(par, first, src_ap, src_dep):
            xt, uT, zs, gate, lg = xt_[par], uT_[par], zs_[par], gate_[par], lg_[par]
            lab = "FA%d" % S.tilecount
            S.tilecount += 1

            def _lab(g):
                while True:
                    S.label = lab
                    try:
                        next(g)
                    except StopIteration:
                        S.label = None
                        return
                    S.label = None
                    yield
            yield from _lab(front_a_body(par, first, src_ap, src_dep, xt, uT, zs, gate, lg))

        def front_a_body(par, first, src_ap, src_dep, xt, uT, zs, gate, lg):
            load([xt.r()], xt.t[:], src_ap, "xt%d" % par, reads=src_dep)
            (ssq, ssq_r), (rstd, rstd_r) = stc(ST_SSQ), stc(ST_RSTD)
            dve(mset(st8.t[:, 0:2], 0.0), (), [st8.r(0, 2)])
            act(af(xb.t[:], xt.t[:], AF.Square, accum=ssq), [xt.r()], [xb.r(), ssq_r])
            dve(ts(rstd, ssq, 1.0 / D, ALU.mult, EPS, ALU.add), [ssq_r], [rstd_r])
            act(af(rstd, rstd, AF.Sqrt), [rstd_r], [rstd_r])
            dve(lambda e: e.reciprocal(out=rstd, in_=rstd), [rstd_r], [rstd_r])
            act(af(xb.t[:], xt.t[:], AF.Copy, scale=rstd), [xt.r(), rstd_r], [xb.r()])
            yield
            for kc in range(8):
                pe(tr(BT.t[:, kc * 128:(kc + 1) * 128], xb.t[:, kc * 128:(kc + 1) * 128], identb.t[:]), [xb.r(), identb.r()],
                   [BT.r()], sig=(kc == 7))
            act(af(hT.t[:], BT.t[:], AF.Copy), [BT.r()], [hT.r()])
            yield
            bank = B[6]
            pT3 = pT.t[:].rearrange("p (j t) -> p j t", t=131)
            if first:
                pool(mset(pT.t[:], 0.0), (), [pT.r()])

            def win_fm(col0):
                for j in range(4):
                    for kc in range(8):
                        c = kc * INC + col0 + j * 128
                        pe(mm(bank.t[:, j * 128:(j + 1) * 128], win_b.t[:, c:c + 128], hT.t[:, kc * 128:(kc + 1) * 128],
                              start=(kc == 0), stop=(kc == 7)), [win_b.r(), hT.r()], [bank.r()],
                           sig=(j == 3 and kc == 7))
                    yield
            for g3 in range(3):
                yield from win_fm(g3 * 512)
                act(af(pT3[:, 4 * g3:4 * g3 + 4, 3:131], bank.t[:].rearrange("p (j t) -> p j t", t=128), AF.Copy),
                    [bank.r()], [pT.r()])
            yield from win_fm(2056)
            act(af(uT.t[:], bank.t[:], AF.Copy), [bank.r()], [uT.r()])
            yield from win_fm(2568)
            act(af(zs.t[:], bank.t[:], AF.Silu), [bank.r()], [zs.r()])
            for kc in range(8):
                pe(mm(bank.t[:, :], hT.t[:, kc * 128:(kc + 1) * 128], win_b.t[:, kc * INC + 1536: kc * INC + 2048],
                      start=(kc == 0), stop=(kc == 7)), [win_b.r(), hT.r()], [bank.r()], sig=(kc == 7))
                if kc % 4 == 3:
                    yield
            act(af(gate.t[:], bank.t[:], AF.Silu), [bank.r()], [gate.r()])
            for kc in range(8):
                pe(mm(bank.t[:, 0:8], hT.t[:, kc * 128:(kc + 1) * 128], win_b.t[:, kc * INC + 2048: kc * INC + 2056],
                      start=(kc == 0), stop=(kc == 7)), [win_b.r(), hT.r()], [bank.r()], sig=(kc == 7))
            act(af(lg.t[:], bank.t[:, 0:8], AF.Copy), [bank.r()], [lg.r()])
            yield

        CB = [B[2], B[3], B[6]]

        (beta, beta_r), (gx, gx_r), (gna, gna_r), (ge, ge_r), (gl_, gl_r), (gsp, gsp_r), (gg, gg_r) = \
            gt(G_BETA), gt(G_X), gt(G_NA), gt(G_E), gt(G_L), gt(G_SP), gt(G_G)
        (gc, gc_r), (glast, glast_r), (bg, bg_r), (kd, kd_r), (gtmp, gtmp_r) = gt(G_GC), gt(G_GL), gt(G_BG), gt(G_KD), gt(G_T)

        def b3(ap):
            return ap.unsqueeze(2).to_broadcast([128, 4, 128])

        def front_b(par, first, dbg):
            xt, uT, zs, gate, lg = xt_[par], uT_[par], zs_[par], gate_[par], lg_[par]
            pT3 = pT.t[:].rearrange("p (j t) -> p j t", t=131)
            act(af(beta, lg.t[:, 0:4], AF.Sigmoid), [lg.r()], [beta_r])
            dve(tt(gx, lg.t[:, 4:8], dtb.t[:], ALU.add), [lg.r(), dtb.r()], [gx_r])
            dve(stt(gna, gx, -1.0, gx, ALU.mult, ALU.min), [gx_r], [gna_r])
            act(af(ge, gna, AF.Exp), [gna_r], [ge_r])
            act(af(gl_, ge, AF.Ln, bias=1.0), [ge_r], [gl_r])
            dve(stt(gsp, gx, 0.0, gl_, ALU.max, ALU.add), [gx_r, gl_r], [gsp_r])
            dve(stt(gg, gsp, -1.0, ealog.t[:], ALU.mult, ALU.mult), [gsp_r, ealog.r()], [gg_r])
            yield

            for j in range(12):
                bank = CB[j // 4]
                jj = j % 4
                for k in range(4):
                    i = j * 4 + k
                    pe(mm(bank.t[:, jj * 128:(jj + 1) * 128], cdiag.t[:, i * 128:(i + 1) * 128], pT.t[:, j * 131 + k: j * 131 + k + 128],
                          start=(k == 0), stop=(k == 3)), [cdiag.r(), pT.r()], [bank.r()], sig=(jj == 3 and k == 3))
            act(af(qks.t[:, 0:512], CB[0].t[:], AF.Silu), [CB[0].r()], [qks.r()])
            act(af(qks.t[:, 512:1024], CB[1].t[:], AF.Silu), [CB[1].r()], [qks.r()])
            act(af(vT.t[:], CB[2].t[:], AF.Silu), [CB[2].r()], [vT.r()])
            pool(cp(pT3[:, :, 0:3], pT3[:, :, 128:131]), [pT.r()], [pT.r()])
            yield

            pool(tt(sq.t[:], qks.t[:], qks.t[:], ALU.mult), [qks.r()], [sq.r()])
            pe(mm(CB[0].t[:], onesb.t[:], sq.t[:, 0:512]), [onesb.r(), sq.r()], [CB[0].r()])
            pe(mm(CB[1].t[:], onesb.t[:], sq.t[:, 512:1024]), [onesb.r(), sq.r()], [CB[1].r()])
            yield
            act(af(rinv.t[:, 0:512], CB[0].t[:], AF.Sqrt, scale=128.0, bias=128.0 * EPS), [CB[0].r()], [rinv.r()])
            act(af(rinv.t[:, 512:1024], CB[1].t[:], AF.Sqrt, bias=EPS), [CB[1].r()], [rinv.r()])
            dve(lambda e: e.reciprocal(out=rinv.t[:], in_=rinv.t[:]), [rinv.r()], [rinv.r()])
            dve(tt(qkn.t[:], qks.t[:], rinv.t[:], ALU.mult), [qks.r(), rinv.r()], [qkn.r()])
            yield

            pe(mm(CB[2].t[:, 8:12], tri.t[:], gg), [tri.r(), gg_r], [CB[2].r()], sig=False)
            pe(mm(CB[2].t[:, 12:16], blkm.t[:], gg), [blkm.r(), gg_r], [CB[2].r()])
            dve(cp(gts.t[:, 4 * G_GC:4 * G_GC + 8], CB[2].t[:, 8:16]), [CB[2].r()], [gc_r, glast_r])
            yield
            for h in range(NH):
                dve(ts(gh.t[:, h * 128:(h + 1) * 128], tri.t[:], gts.t[:, 4 * G_G + h:4 * G_G + h + 1], ALU.mult),
                    [tri.r(), gg_r], [gh.r()])
            for h in range(NH):
                pe(mm(CB[0].t[:, h * 128:(h + 1) * 128], onesf.t[:], gh.t[:, h * 128:(h + 1) * 128]), [onesf.r(), gh.r()],
                   [CB[0].r()], sig=(h == NH - 1))
            act(af(gcrow.t[:], CB[0].t[:], AF.Copy), [CB[0].r()], [gcrow.r()])
            act(af(egrow.t[:], gcrow.t[:], AF.Exp), [gcrow.r()], [egrow.r()])
            yield
            dve(tt(qgT.t[:], qkn.t[:, 0:512], egrow.t[:], ALU.mult), [qkn.r(), egrow.r()], [qgT.r()])
            act(af(gtmp, gc, AF.Exp), [gc_r], [gtmp_r])
            dve(tt(bg, beta, gtmp, ALU.mult), [beta_r, gtmp_r], [bg_r])
            dve(tt(kd, glast, gc, ALU.subtract), [glast_r, gc_r], [kd_r])
            act(af(kd, kd, AF.Exp), [kd_r], [kd_r])
            yield
            for h in range(NH):
                pe(tr(BT.t[:, h * 128:(h + 1) * 128], qkn.t[:, 512 + h * 128: 512 + (h + 1) * 128], identb.t[:]),
                   [qkn.r(), identb.r()], [BT.r()], sig=False)
            for h in range(NH):
                pe(tr(BT.t[:, 512 + h * 128:512 + (h + 1) * 128], vT.t[:, h * 128:(h + 1) * 128], identb.t[:]),
                   [vT.r(), identb.r()], [BT.r()], sig=(h == NH - 1))
            k3 = BT.t[:, 0:512].rearrange("p (h d) -> p h d", d=128)
            v3 = BT.t[:, 512:1024].rearrange("p (h d) -> p h d", d=128)

            dve(tt(kbg.t[:].rearrange("p (h d) -> p h d", d=128), k3, b3(bg), ALU.mult), [BT.r(), bg_r], [kbg.r()])
            dve(tt(kdec.t[:].rearrange("p (h d) -> p h d", d=128), k3, b3(kd), ALU.mult), [BT.r(), kd_r], [kdec.r()])
            dve(tt(vb.t[:].rearrange("p (h d) -> p h d", d=128), v3, b3(beta), ALU.mult), [BT.r(), beta_r], [vb.r()])
            yield

            if first:
                dve(mset(s_f.t[:], 0.0), (), [s_f.r()])
                dve(mset(s_b.t[:], 0.0), (), [s_b.r()])
                dve(mset(s5st.t[:], 0.0), (), [s5st.r()])

            if "pre" in dbg:
                tap("qkn", qkn.t[:], 1024, BF16, [qkn.r()])
                tap("vT", vT.t[:], 512, BF16, [vT.r()])
                tap("gts", gts.t[:], 64, F32, [gts.r()])
                tap("gcrow", gcrow.t[:], 512, F32, [gcrow.r()])
                tap("uT", uT.t[:], 512, BF16, [uT.r()])
                tap("ealog", ealog.t[:], 4, F32, [ealog.r()])
                tap("alog", alog.t[:], 4, F32, [alog.r()])


        def mixers(par, nxt_gen, curt, dbg):
            xt, uT, zs, gate, lg = xt_[par], uT_[par], zs_[par], gate_[par], lg_[par]
            def gdn_head(h):
                g_ = gd[h]
                Bk = B[h]
                hs = slice(h * 128, (h + 1) * 128)
                kT_h = qkn.t[:, 512 + h * 128: 512 + (h + 1) * 128]
                qT_h = qkn.t[:, hs]
                S0, S1, S2, S3 = (Bk.t[:, i * 128:(i + 1) * 128] for i in range(4))
                X, XT, PT = g_["x"], g_["xT"], g_["pt"]
                bk = [Bk.r()]
                pe(mm(S0, kT_h, kT_h), [qkn.r()], bk, sig=False)
                pe(mm(S1, kT_h, qT_h), [qkn.r()], bk)
                yield
                gcol = gts.t[:, 4 * G_GC + h:4 * G_GC + h + 1]
                dve(stt(X.t[:], gcrow.t[:, hs], gcol, maska.t[:], ALU.subtract, ALU.max), [gcrow.r(), gc_r, maska.r()], [X.r()])
                act(af(X.t[:], X.t[:], AF.Exp, scale=-1.0), [X.r()], [X.r()])
                dve(stt(XT.t[:], gcrow.t[:, hs], gcol, maski.t[:], ALU.subtract, ALU.min), [gcrow.r(), gc_r, maski.r()], [XT.r()])
                act(af(XT.t[:], XT.t[:], AF.Exp), [XT.r()], [XT.r()])
                yield
                dve(stt(X.t[:], S0, gts.t[:, 4 * G_BETA + h:4 * G_BETA + h + 1], X.t[:], ALU.mult, ALU.mult),
                    bk + [beta_r, X.r()], [X.r()])
                dve(tt(g_["intra"].t[:], S1, XT.t[:], ALU.mult), bk + [XT.r()], [g_["intra"].r()])
                yield
                pe(mm(S2, X.t[:], identf.t[:]), [X.r(), identf.r()], bk)
                yield
                dve(cp(XT.t[:], S2), bk, [XT.r()])
                dve(tt(PT.t[:], identf.t[:], S2, ALU.subtract), [identf.r()] + bk, [PT.r()])
                yield
                for lev in range(1, 6):
                    pe(mm(S0, XT.t[:], X.t[:]), [XT.r(), X.r()], bk, sig=(lev == 5))
                    if lev < 5:
                        pe(mm(S1, X.t[:], XT.t[:]), [XT.r(), X.r()], bk)
                    yield
                    dve(cp(X.t[:], S0), bk, [X.r()])
                    if lev < 5:
                        act(af(XT.t[:], S1, AF.Copy), bk, [XT.r()])
                    yield
                    pe(mm(S2, X.t[:], PT.t[:]), [X.r(), PT.r()], bk)
                    yield
                    if lev < 5:
                        dve(tt(PT.t[:], PT.t[:], S2, ALU.add), [PT.r()] + bk, [PT.r()])
                    else:
                        dve(tt(g_["ttb"].t[:], PT.t[:], S2, ALU.add), [PT.r()] + bk, [g_["ttb"].r()])
                    yield
                pe(mm(S0, kbg.t[:, hs], g_["ttb"].t[:]), [kbg.r(), g_["ttb"].r()], bk, sig=False)
                pe(mm(S1, g_["ttb"].t[:], vb.t[:, hs]), [vb.r(), g_["ttb"].r()], bk)
                yield
                act(af(g_["wtb"].t[:], S0, AF.Copy), bk, [g_["wtb"].r()])
                dve(cp(g_["usb"].t[:], S1), bk, [g_["usb"].r()])
                yield
                sbr = s_b.r(h * 128, h * 128 + 128)
                sfr = s_f.r(h * 128, h * 128 + 128)
                for j in range(2):
                    r0, r1 = 64 * j, 64 * j + 64
                    pe(mm(Bk.t[r0:r1, 256:384], g_["wtb"].t[:, r0:r1], s_b.t[:, hs]), [g_["wtb"].r(), sbr], bk)
                    yield
                    dve(tt(g_["vn"].t[r0:r1, :], g_["usb"].t[r0:r1, :], Bk.t[r0:r1, 256:384], ALU.subtract),
                        [g_["usb"].r()] + bk, [g_["vn"].r()])
                    yield
                    pe(mm(Bk.t[r0:r1, 384:512], qgT.t[:, h * 128 + r0:h * 128 + r1], s_b.t[:, hs], start=True, stop=False),
                       [qgT.r(), sbr], bk, sig=False)
                    pe(mm(Bk.t[r0:r1, 384:512], g_["intra"].t[r0:r1, r0:r1], g_["vn"].t[r0:r1, :], start=False, stop=True),
                       [g_["intra"].r(), g_["vn"].r()], bk, sig=False)
                    pe(mm(S2, kdec.t[r0:r1, hs], g_["vn"].t[r0:r1, :]), [kdec.r(), g_["vn"].r()], bk)
                    yield
                    egl = egrow.t[:, h * 128 + r1 - 1:h * 128 + r1]
                    dve(stt(s_f.t[:, hs], s_f.t[:, hs], egl, S2, ALU.mult, ALU.add), [sfr, egrow.r()] + bk, [sfr])
                    act(af(s_b.t[:, hs], s_f.t[:, hs], AF.Copy), [sfr], [sbr])
                    yield
                act(af(o_sb.t[:, hs], S3, AF.Copy), bk, [o_sb.r(h * 128, h * 128 + 128)])

            Y = B[5]

            def s5_ct(ct):
                V = B[4]
                cosT = tabc.t[:, ct * 512:(ct + 1) * 512]
                sinT = tabs.t[:, ct * 512:(ct + 1) * 512]
                t1, t2, t3, t4 = tmp
                for pm in range(4):
                    pair = 4 * ct + pm
                    pe(mm(V.t[:, pm * 128:(pm + 1) * 128], bpad.t[:, (2 * pair) * 128:(2 * pair + 1) * 128], uT.t[:, ct * 128:(ct + 1) * 128]),
                       [bpad.r(), uT.r()], [V.r()], sig=(pm == 3))
                yield
                dve(tt(t1.t[:], V.t[:], cosT, ALU.mult), [V.r(), tabc.r()], [t1.r()])
                dve(tt(t2.t[:], V.t[:], sinT, ALU.mult), [V.r(), tabs.r()], [t2.r()])
                yield
                for pm in range(4):
                    pair = 4 * ct + pm
                    pe(mm(V.t[:, pm * 128:(pm + 1) * 128], bpad.t[:, (2 * pair + 1) * 128:(2 * pair + 2) * 128], uT.t[:, ct * 128:(ct + 1) * 128]),
                       [bpad.r(), uT.r()], [V.r()], sig=(pm == 3))
                yield
                dve(tt(t3.t[:], V.t[:], sinT, ALU.mult), [V.r(), tabs.r()], [t3.r()])
                dve(tt(t4.t[:], V.t[:], cosT, ALU.mult), [V.r(), tabc.r()], [t4.r()])
                yield
                dve(tt(t1.t[:], t1.t[:], t3.t[:], ALU.add), [t1.r(), t3.r()], [t1.r()])
                dve(tt(t2.t[:], t4.t[:], t2.t[:], ALU.subtract), [t2.r(), t4.r()], [t2.r()])
                yield
                for pm in range(4):
                    pair = 4 * ct + pm
                    rb = rmag.t[:, pair:pair + 1].to_broadcast([128, 128])
                    dve(scan(t3.t[:, pm * 128:(pm + 1) * 128], rb, t1.t[:, pm * 128:(pm + 1) * 128], s5st.t[:, pair:pair + 1]),
                        [rmag.r(), t1.r(), s5st.r()], [t3.r()])
                    yield
                for pm in range(4):
                    pair = 4 * ct + pm
                    rb = rmag.t[:, pair:pair + 1].to_broadcast([128, 128])
                    dve(scan(t4.t[:, pm * 128:(pm + 1) * 128], rb, t2.t[:, pm * 128:(pm + 1) * 128], s5st.t[:, 16 + pair:17 + pair]),
                        [rmag.r(), t2.r(), s5st.r()], [t4.r()])
                    yield
                dve(tt(t1.t[:], t3.t[:], cosT, ALU.mult), [t3.r(), tabc.r()], [t1.r()])
                dve(tt(t2.t[:], t4.t[:], sinT, ALU.mult), [t4.r(), tabs.r()], [t2.r()])
                dve(tt(sre_b.t[:], t1.t[:], t2.t[:], ALU.subtract), [t1.r(), t2.r()], [sre_b.r()])
                yield
                l1 = t1.t[:].rearrange("p (a t) -> p a t", t=128)[:, :, 127:128]
                l2 = t2.t[:].rearrange("p (a t) -> p a t", t=128)[:, :, 127:128]
                dve(tt(s5st.t[:, 4 * ct:4 * ct + 4].unsqueeze(2), l1, l2, ALU.subtract), [t1.r(), t2.r()], [s5st.r(0, 16)])
                yield
                dve(tt(t1.t[:], t3.t[:], sinT, ALU.mult), [t3.r(), tabs.r()], [t1.r()])
                dve(tt(t2.t[:], t4.t[:], cosT, ALU.mult), [t4.r(), tabc.r()], [t2.r()])
                dve(tt(sim_b.t[:], t1.t[:], t2.t[:], ALU.add), [t1.r(), t2.r()], [sim_b.r()])
                yield
                dve(tt(s5st.t[:, 16 + 4 * ct:16 + 4 * ct + 4].unsqueeze(2), l1, l2, ALU.add), [t1.r(), t2.r()], [s5st.r(16, 32)])
                yield
                for pm in range(4):
                    pair = 4 * ct + pm
                    pe(mm(Y.t[:, ct * 128:(ct + 1) * 128], cpad.t[:, (2 * pair) * 128:(2 * pair + 1) * 128], sre_b.t[:, pm * 128:(pm + 1) * 128],
                          start=(pm == 0), stop=False), [cpad.r(), sre_b.r()], [Y.r()], sig=False)
                    pe(mm(Y.t[:, ct * 128:(ct + 1) * 128], cpad.t[:, (2 * pair + 1) * 128:(2 * pair + 2) * 128], sim_b.t[:, pm * 128:(pm + 1) * 128],
                          start=False, stop=(pm == 3)), [cpad.r(), sim_b.r()], [Y.r()], sig=(pm == 3))

            def chain(*gens):
                for g in gens:
                    yield from g

            def run_rr(gens):
                gens = list(gens)
                while gens:
                    for g in list(gens):
                        try:
                            next(g)
                        except StopIteration:
                            gens.remove(g)

            def onorm():
                (ossq, ossq_r), (orstd, orstd_r) = stc(ST_OSSQ, 4), stc(ST_ORSTD, 4)
                for h in range(NH):
                    act(af(junk.t[:, 0:128], o_sb.t[:, h * 128:(h + 1) * 128], AF.Square, accum=st8.t[:, ST_OSSQ + h:ST_OSSQ + h + 1]),
                        [o_sb.r()], [junk.r(), ossq_r])
                dve(ts(orstd, ossq, 1.0 / 128, ALU.mult, EPS, ALU.add), [ossq_r], [orstd_r])
                act(af(orstd, orstd, AF.Sqrt), [orstd_r], [orstd_r])
                dve(lambda e: e.reciprocal(out=orstd, in_=orstd), [orstd_r], [orstd_r])
                yield
                o3 = o_sb.t[:].rearrange("p (h d) -> p h d", d=128)
                dve(tt(o3, o3, b3(orstd), ALU.mult), [o_sb.r(), orstd_r], [o_sb.r()])
                dve(tt(o3, o3, normg.t[:].unsqueeze(1).to_broadcast([128, 4, 128]), ALU.mult), [o_sb.r(), normg.r()], [o_sb.r()])
                dve(tt(odn.t[:], o_sb.t[:], gate.t[:], ALU.mult), [o_sb.r(), gate.r()], [odn.r()])
                yield
                for h in range(NH):
                    pe(tr(BT.t[:, h * 128:(h + 1) * 128], odn.t[:, h * 128:(h + 1) * 128], identb.t[:]), [odn.r(), identb.r()], [BT.r()],
                       sig=(h == NH - 1))
                act(af(mixT.t[:, 0:512], BT.t[:, 0:512], AF.Copy), [BT.r()], [mixT.r(0, 512)])
                yield
                if "gdn" in dbg:
                    tap("odn", odn.t[:], 512, BF16, [odn.r()])
                    tap("osb", o_sb.t[:], 512, F32, [o_sb.r()])
                yield

            def run_phase(heads, s5chain):
                heads = list(heads)
                labels = {id(g): "GDN%d" % curt for g in heads}
                labels[id(s5chain)] = "S5_%d" % curt
                tail = onorm()
                labels[id(tail)] = "ON%d" % curt
                live = {"s5": s5chain, "nx": nxt_gen, "tail": None}

                def step(g):
                    S.label = labels.get(id(g))
                    try:
                        next(g)
                        return True
                    except StopIteration:
                        return False
                    finally:
                        S.label = None
                while heads or live["s5"] is not None or live["tail"] is not None:
                    for g in list(heads):
                        if not step(g):
                            heads.remove(g)
                    if not heads and live["tail"] is None and tail is not None:
                        live["tail"] = tail
                        tail = None
                    elif live["tail"] is not None:
                        if not step(live["tail"]):
                            live["tail"] = None
                    for _ in range(S5_STEPS):
                        if live["s5"] is not None and not step(live["s5"]):
                            live["s5"] = None
                    if live["nx"] is not None and not step(live["nx"]):
                        live["nx"] = None

            run_phase([gdn_head(0), gdn_head(1), gdn_head(2), gdn_head(3)], chain(s5_ct(0), s5_ct(1), s5_ct(2), s5_ct(3)))
            if nxt_gen is not None:
                for _ in nxt_gen:
                    pass

        def epilogue(par, dst_ap, dst_regs, dbg, res):
            xt, uT, zs, gate, lg = xt_[par], uT_[par], zs_[par], gate_[par], lg_[par]
            Y = B[5]
            dve(mset(st8.t[:, 2:16], 0.0), (), [st8.r(2, 16)])
            for ct in range(4):
                cs_ = slice(ct * 128, (ct + 1) * 128)
                dve(stt(y_sb.t[:, cs_], uT.t[:, cs_], ssmd.t[:, ct:ct + 1], Y.t[:, cs_], ALU.mult, ALU.add), [uT.r(), ssmd.r(), Y.r()],
                    [y_sb.r()])
            act(af(gy.t[:], y_sb.t[:], AF.Gelu_apprx_tanh if GELU_TANH else AF.Gelu), [y_sb.r()], [gy.r()])
            yield
            if "s5" in dbg:
                tap("ysb", y_sb.t[:], 512, F32, [y_sb.r()])
            for half, bank in ((0, B[4]), (1, B[5])):
                for j in range(4):
                    for kc in range(4):
                        c = kc * 1024 + half * 512 + j * 128
                        pe(mm(bank.t[:, j * 128:(j + 1) * 128], glu_b16.t[:, c:c + 128], gy.t[:, kc * 128:(kc + 1) * 128],
                              start=(kc == 0), stop=(kc == 3)), [glu_b16.r(), gy.r()], [bank.r()], sig=(j == 3 and kc == 3))
                    yield
            for j in range(4):
                act(af(sg.t[:, j * 128:(j + 1) * 128], B[5].t[:, j * 128:(j + 1) * 128], AF.Sigmoid, bias=glub.t[:, 4 + j:5 + j]),
                    [B[5].r(), glub.r()], [sg.r()])
            dve(tt(sg.t[:], sg.t[:], zs.t[:], ALU.mult), [sg.r(), zs.r()], [sg.r()])
            yield
            for j in range(4):
                dve(stt(mixT.t[:, 512 + j * 128:512 + (j + 1) * 128], B[4].t[:, j * 128:(j + 1) * 128], glub.t[:, j:j + 1],
                        sg.t[:, j * 128:(j + 1) * 128], ALU.add, ALU.mult), [B[4].r(), glub.r(), sg.r()], [mixT.r(512, 1024)])

            for half in range(2):
                for kc in range(8):
                    pe(mm(B[half].t[:], mixT.t[:, kc * 128:(kc + 1) * 128], wout_b.t[:, kc * 1024 + half * 512: kc * 1024 + half * 512 + 512],
                          start=(kc == 0), stop=(kc == 7)), [mixT.r(), wout_b.r()], [B[half].r()], sig=(kc == 7))
                    if kc % 4 == 3:
                        yield
            (sa, sa_r), (sb_, sb_r), (r2, r2_r) = stc(ST_SSQ2A), stc(ST_SSQ2B), stc(ST_RSTD2)
            for half in range(2 if NEW_TAIL else 0):
                hs_ = slice(half * 512, (half + 1) * 512)
                dve(tt(xo.t[:, hs_], B[half].t[:], gpost.t[:, hs_], ALU.mult), [B[half].r(), gpost.r()], [xo.r()])
            act(af(junk.t[:, 0:512], B[0].t[:], AF.Square, accum=sa), [B[0].r()], [junk.r(), sa_r])
            act(af(junk.t[:, 512:1024], B[1].t[:], AF.Square, accum=sb_), [B[1].r()], [junk.r(), sb_r])
            dve(tt(r2, sa, sb_, ALU.add), [sa_r, sb_r], [r2_r])
            dve(ts(r2, r2, 1.0 / D, ALU.mult, EPS, ALU.add), [r2_r], [r2_r])
            act(af(r2, r2, AF.Sqrt), [r2_r], [r2_r])
            dve(lambda e: e.reciprocal(out=r2, in_=r2), [r2_r], [r2_r])
            yield
            if NEW_TAIL:
                dve(stt(xo.t[:], xo.t[:], r2, xt.t[:], ALU.mult, ALU.add), [xo.r(), r2_r, xt.r()], [xo.r()])
            else:
                for half in range(2):
                    hs_ = slice(half * 512, (half + 1) * 512)
                    dve(stt(xo.t[:, hs_], B[half].t[:], r2, gpost.t[:, hs_], ALU.mult, ALU.mult), [B[half].r(), r2_r, gpost.r()], [xo.r()])
                pool(tt(xo.t[:], xo.t[:], xt.t[:], ALU.add), [xo.r(), xt.r()], [xo.r()])
            res["tok"] = S.op("pool", dma(dst_ap, xo.t[:]), [xo.r()], dst_regs, dma_sem=S.dsem("xo"))

            yield

        def rr(gens, labels, reps=None):
            gens = [g for g in gens if g is not None]
            reps = reps or {}
            while gens:
                for g in list(gens):
                    S.label = labels.get(id(g))
                    for _ in range(reps.get(id(g), 1)):
                        try:
                            next(g)
                        except StopIteration:
                            gens.remove(g)
                            break
            S.label = None

        last = None
        for l in range(nlayers):
            layer_setup(l)
            src = x_d if l == 0 else x1_t
            dst = out_d if l == nlayers - 1 else x1_t
            tiles = [(s, it) for s in range(nseq) for it in range(ntile)]

            def mk_front(idx):
                s, it = tiles[idx]
                ti = s * ntile + it
                rows = slice(ti * T, (ti + 1) * T)
                src_dep = [(x1_b, ti, ti + 1)] if l > 0 else []
                return front_a(idx % 2, it == 0, src[rows, :], src_dep)

            def mk_fb(idx):
                s, it = tiles[idx]
                dbg = {k for k, v in debug.items() if v == (l, s, it)}
                return front_b(idx % 2, it == 0, dbg)
            for _ in mk_front(0):
                pass
            g0 = mk_fb(0)
            rr([g0], {id(g0): "FB0"})
            for idx, (s, it) in enumerate(tiles):
                ti = s * ntile + it
                rows = slice(ti * T, (ti + 1) * T)
                dst_regs = [(x1_b, ti, ti + 1)] if l < nlayers - 1 else []
                nxt = mk_front(idx + 1) if idx + 1 < len(tiles) else None
                dbg = {k for k, v in debug.items() if v == (l, s, it)}
                mixers(idx % 2, nxt, idx, dbg)
                res = {}
                ge_ = epilogue(idx % 2, dst[rows, :], dst_regs, dbg, res)
                gf_ = mk_fb(idx + 1) if idx + 1 < len(tiles) else None
                rr([ge_, gf_], {id(ge_): "EP%d" % idx, **({id(gf_): "FB%d" % (idx + 1)} if gf_ is not None else {})}, {id(ge_): EP_STEPS})
                last = res["tok"]
        fin = [last] + list(dbg_out.values())
        S.final_wait("pool", fin)
        if VERBOSE:
            print("instr counts", {e: (len(S.streams[e]), sum(len(w) for w, _, _, _, _ in S.streams[e])) for e in S.ENG}, flush=True)
        S.emit(block)
    return nc


GELU_TANH = True
NEW_TAIL = True
S5_STEPS = 2
EP_STEPS = 2
ANNOTATE = False
VERBOSE = False


def prep_params(p, nlayers):
    f = lambda a: np.ascontiguousarray(np.asarray(a, dtype=np.float32))
    L = nlayers
    out = {}
    out["w_in"] = f(p["w_in"][:L])
    out["w_out"] = f(p["w_out"][:L])
    out["glu_w"] = f(p["glu_w"][:L])
    out["preg"] = f(np.asarray(p["pre_norm_g"])[:L].reshape(L, 8, 128).transpose(0, 2, 1))
    out["postg"] = f(np.broadcast_to(np.asarray(p["post_norm_g"])[:L, None, :], (L, 128, 1024)))
    out["convw"] = f(np.asarray(p["conv_w"])[:L].reshape(L, 4, 12, 128).transpose(0, 3, 2, 1).reshape(L, 128, 48))
    out["alog"] = f(np.broadcast_to(np.asarray(p["dn_a_log"])[:L, None, :], (L, 128, 4)))
    out["dtb"] = f(np.broadcast_to(np.asarray(p["dn_dt_bias"])[:L, None, :], (L, 128, 4)))
    out["normg"] = f(np.broadcast_to(np.asarray(p["dn_norm_g"])[:L, None, :], (L, 128, 128)))
    out["glub"] = f(np.asarray(p["glu_b"])[:L].reshape(L, 8, 128).transpose(0, 2, 1))
    out["ssmd"] = f(np.asarray(p["ssm_d"])[:L].reshape(L, 4, 128).transpose(0, 2, 1))

    def gp(a):
        return f(np.asarray(a)[:L].reshape(L, 16, 2, 64).transpose(0, 2, 3, 1).reshape(L, 128, 16))
    out["are"] = gp(p["ssm_a_re"])
    out["aim"] = gp(p["ssm_a_im"])
    out["ldt"] = gp(np.broadcast_to(np.asarray(p["ssm_log_dt"])[:L, :, None], (L, 32, 64)))

    def gb(a):
        return f(np.asarray(a)[:L].reshape(L, 16, 2, 64, 16).transpose(0, 2, 3, 1, 4).reshape(L, 128, 256))
    out["bre"] = gb(p["ssm_b_re"])
    out["bim"] = gb(p["ssm_b_im"])
    out["cre"] = gb(np.asarray(p["ssm_c_re"]).transpose(0, 1, 3, 2))
    out["cim"] = gb(np.asarray(p["ssm_c_im"]).transpose(0, 1, 3, 2))
    i = np.arange(128)
    same = (i[:, None] // 64) == (i[None, :] // 64)
    out["identf"] = np.eye(128, dtype=np.float32)
    out["tri"] = ((i[:, None] <= i[None, :]) & same).astype(np.float32)
    out["blkm"] = same.astype(np.float32)
    out["maska"] = np.where((i[None, :] < i[:, None]) & same, 0.0, 1e4).astype(np.float32)
    out["maski"] = np.where((i[:, None] <= i[None, :]) & same, 0.0, -1e4).astype(np.float32)
    return out


_CACHE = {}


def kernel(**inputs):
    x = np.asarray(inputs["x"], dtype=np.float32)
    bsz, slen, d = x.shape
    nlayers = np.asarray(inputs["w_in"]).shape[0]
    nseq = bsz // NCORES
    key = (nseq, slen, nlayers)
    if key not in _CACHE:
        _CACHE[key] = build_program(nseq, slen, nlayers)
    nc = _CACHE[key]
    params = prep_params(inputs, nlayers)
    in_maps = []
    for c in range(NCORES):
        m = dict(params)
        m["x"] = np.ascontiguousarray(x[c * nseq:(c + 1) * nseq].reshape(nseq * slen, d))
        in_maps.append(m)
    res = run_bass_kernel_spmd(nc, in_maps, core_ids=list(range(NCORES)))
    outs = [np.asarray(r["out"]).reshape(nseq, slen, d) for r in res.results]
    return np.concatenate(outs, axis=0).astype(np.float32)
```

```python
import numpy as np
from contextlib import ExitStack
import ml_dtypes
import concourse.bass as bass
import concourse.mybir as mybir
from concourse.bass_utils import run_bass_kernel_spmd

F32 = mybir.dt.float32
BF16 = mybir.dt.bfloat16
AF = mybir.ActivationFunctionType
ALU = mybir.AluOpType

D = 1024
INC = 3080
NH = 4
EPS = 1e-6
T = 128
NCORES = 8


class Buf:
    def __init__(self, name, t, blk, nblk, psum=False):
        self.name, self.t, self.blk, self.nblk, self.psum = name, t, blk, nblk, psum

    def r(self, lo=0, hi=None):
        if hi is None:
            hi = self.blk * self.nblk
        return (self, lo, hi)


class Sub:
    def __init__(self, parent, off, cols):
        self.parent, self.off, self.cols = parent, off, cols
        self.t = parent.t[:, off:off + cols]
        self.name = parent.name

    def r(self, lo=0, hi=None):
        if hi is None:
            hi = self.cols
        return (self.parent, self.off + lo, self.off + hi)


class Sched:
    ENG = ("pe", "act", "dve", "pool", "sp")

    def __init__(self, sems):
        self.sems = sems
        self.cnt = {}
        self.streams = {e: [] for e in self.ENG}
        self.lastw = {}
        self.readers = {}
        self.seen = {e: {} for e in self.ENG}
        self.free_dma = sorted([k for k in sems if k.startswith("dma")], key=lambda s: int(s[3:]))
        self.bufsem = {}
        self.pending = {e: False for e in self.ENG}
        self.group = None
        self.label = None

    def _blocks(self, reg):
        b, lo, hi = reg
        return [(b.name, i) for i in range(lo // b.blk, (hi - 1) // b.blk + 1)]

    def _need(self, eng, waits, dep):
        if dep is None:
            return
        sem, val, deng, big = dep
        if deng == eng and not sem.startswith("dma") and (eng == "pe" or big):
            return
        if self.seen[eng].get(sem, 0) >= val:
            return
        waits[sem] = max(waits.get(sem, 0), val)

    def op(self, eng, fn, reads=(), writes=(), dma_sem=None, sig=True):
        reads = list(reads)
        writes = list(writes)
        writes += [r for r in reads if r[0].psum]
        reads = [r for r in reads if not r[0].psum]
        waits = {}
        for reg in reads:
            for k in self._blocks(reg):
                self._need(eng, waits, self.lastw.get(k))
        for reg in writes:
            for k in self._blocks(reg):
                self._need(eng, waits, self.lastw.get(k))
                for sem, (val, deng, rbig) in self.readers.get(k, {}).items():
                    self._need(eng, waits, (sem, val, deng, rbig))
        for sem, val in waits.items():
            self.seen[eng][sem] = max(self.seen[eng].get(sem, 0), val)
        if dma_sem is not None:
            sem, inc = dma_sem, 16
        else:
            sem, inc = "e_" + eng, 1
        if sig:
            val = self.cnt.get(sem, 0) + inc
            self.cnt[sem] = val
            if dma_sem is None:
                self.pending[eng] = False
        else:
            val = self.cnt.get(sem, 0) + inc
            self.pending[eng] = True
            inc = 0
        big = getattr(fn, "n", 0) >= (128 if eng == "dve" else BIG_N) and eng != "pool"
        me = (sem, val, eng, big)
        for reg in reads:
            for k in self._blocks(reg):
                self.readers.setdefault(k, {})[sem] = (val, eng, big)
        for reg in writes:
            for k in self._blocks(reg):
                self.lastw[k] = me
                self.readers[k] = {}
                if self.group is not None and dma_sem is not None:
                    self.group.append(k)
        self.streams[eng].append((list(waits.items()), fn, sem, inc, self.label))
        return me

    def group_begin(self):
        self.group = []

    def group_end(self, sem, eng="sp"):
        fin = (sem, self.cnt.get(sem, 0), eng, False)
        for k in self.group:
            self.lastw[k] = fin
        self.group = None

    def dsem(self, key):
        if key not in self.bufsem:
            self.bufsem[key] = self.free_dma.pop(0)
        return self.bufsem[key]

    def final_wait(self, eng, deps):
        waits = {}
        for d in deps:
            self._need(eng, waits, d)
        self.streams[eng].append((list(waits.items()), None, None, 0, None))

    def emit(self, block):
        sems = self.sems
        for e in self.ENG:
            assert not self.pending[e], "unsignalled tail on " + e

        def play(engobj, name):
            for waits, fn, sem, inc, label in self.streams[name]:
                for s, v in waits:
                    engobj.wait_ge(sems[s], v)
                if fn is not None:
                    ins = fn(engobj)
                    if inc:
                        ins.then_inc(sems[sem], inc)
                    if label is not None and ANNOTATE:
                        ins.annotate(label)

        @block.tensor
        def _(e):
            play(e, "pe")

        @block.scalar
        def _(e):
            play(e, "act")

        @block.vector
        def _(e):
            play(e, "dve")

        @block.gpsimd
        def _(e):
            play(e, "pool")

        @block.sync
        def _(e):
            play(e, "sp")


BIG_N = 512


def _sz(fn, out):
    try:
        fn.n = int(out.free_size())
    except Exception:
        fn.n = 0
    return fn


def mm(out, lhsT, rhs, start=True, stop=True):
    return lambda e: e.matmul(out, lhsT=lhsT, rhs=rhs, start=start, stop=stop)


def tr(out, in_, ident):
    return lambda e: e.transpose(out=out, in_=in_, identity=ident)


def af(out, in_, func, bias=None, scale=None, accum=None):
    kw = {}
    if bias is not None:
        kw["bias"] = bias
    if scale is not None:
        kw["scale"] = scale
    if accum is not None:
        kw["accum_out"] = accum
    return _sz(lambda e: e.activation(out=out, in_=in_, func=func, **kw), out)


def tt(out, a, b, op):
    return _sz(lambda e: e.tensor_tensor(out=out, in0=a, in1=b, op=op), out)


def ts(out, a, s1, op0, s2=None, op1=None):
    if op1 is None:
        return _sz(lambda e: e.tensor_scalar(out=out, in0=a, scalar1=s1, scalar2=None, op0=op0), out)
    return _sz(lambda e: e.tensor_scalar(out=out, in0=a, scalar1=s1, scalar2=s2, op0=op0, op1=op1), out)


def stt(out, a, sc, b, op0, op1):
    return _sz(lambda e: e.scalar_tensor_tensor(out=out, in0=a, scalar=sc, in1=b, op0=op0, op1=op1), out)


def cp(out, in_):
    return _sz(lambda e: e.tensor_copy(out=out, in_=in_), out)


def scan(out, d0, d1, init):
    return lambda e: e.tensor_tensor_scan(out=out, data0=d0, data1=d1, initial=init, op0=ALU.mult, op1=ALU.add)


def dma(out, in_):
    return lambda e: e.dma_start(out=out, in_=in_)


def mset(ap, v):
    return lambda e: e.memset(ap, v)


def build_program(nseq, slen, nlayers, debug=None):
    debug = debug or {}
    ntile = slen // T
    ntok = nseq * slen
    nc = bass.Bass("TRN2", target_bir_lowering=False)
    es = ExitStack()
    dbg_out = {}

    def din(name, shape, dt=F32):
        return nc.dram_tensor(name, list(shape), dt, kind="ExternalInput").ap()

    x_d = din("x", [ntok, D])
    out_d = nc.dram_tensor("out", [ntok, D], F32, kind="ExternalOutput").ap()
    x1_t = nc.dram_tensor("x1", [ntok, D], F32, kind="Internal").ap() if nlayers > 1 else None
    L = nlayers
    w_in_d = din("w_in", [L, D, INC])
    w_out_d = din("w_out", [L, D, D])
    glu_w_d = din("glu_w", [L, 512, 1024])
    preg_d = din("preg", [L, 128, 8])
    postg_d = din("postg", [L, 128, 1024])
    convw_d = din("convw", [L, 128, 48])
    alog_d = din("alog", [L, 128, 4])
    dtb_d = din("dtb", [L, 128, 4])
    normg_d = din("normg", [L, 128, 128])
    glub_d = din("glub", [L, 128, 8])
    ssmd_d = din("ssmd", [L, 128, 4])
    are_d = din("are", [L, 128, 16])
    aim_d = din("aim", [L, 128, 16])
    ldt_d = din("ldt", [L, 128, 16])
    bre_d = din("bre", [L, 128, 256])
    bim_d = din("bim", [L, 128, 256])
    cre_d = din("cre", [L, 128, 256])
    cim_d = din("cim", [L, 128, 256])
    identf_d = din("identf", [128, 128])
    tri_d = din("tri", [128, 128])
    blk_d = din("blkm", [128, 128])
    maska_d = din("maska", [128, 128])
    maski_d = din("maski", [128, 128])

    with es:
        def sb(name, cols, dt=F32, blk=None):
            t = es.enter_context(nc.sbuf_tensor("sb_" + name, [128, cols], dt))
            blk = blk or cols
            return Buf(name, t, blk, cols // blk)

        def ps(name, cols, dt):
            t = es.enter_context(nc.psum_tensor("ps_" + name, [128, cols], dt))
            return Buf(name, t, cols, 1, psum=True)

        class DBuf:
            def __init__(self, name, n):
                self.name, self.blk, self.nblk, self.psum = name, 1, n, False

        x1_b = DBuf("x1d", nseq * ntile)

        identf = sb("identf", 128)
        identb = sb("identb", 128, BF16)
        onesf = sb("onesf", 128)
        onesb = sb("onesb", 128, BF16)
        tri = sb("tri", 128)
        blkm = sb("blkm", 128)
        maska = sb("maska", 128)
        maski = sb("maski", 128)
        win_b = sb("win_b", 8 * INC, BF16)
        wout_b = sb("wout_b", 8 * 1024, BF16)
        glu_b16 = sb("glu_b16", 4 * 1024, BF16)
        SCH = 770
        stage = [sb("stage%d" % i, 1024) for i in range(2)]
        preg = sb("preg", 8)
        gpost = sb("gpost", 1024)
        convw = sb("convw", 48)
        cdiag = sb("cdiag", 48 * 128, BF16)
        alog = sb("alog", 4)
        ealog = sb("ealog", 4)
        dtb = sb("dtb", 4)
        normg = sb("normg", 128)
        glub = sb("glub", 8)
        ssmd = sb("ssmd", 4)
        are = sb("are", 16)
        aim = sb("aim", 16)
        ldt = sb("ldt", 16)
        sm = sb("sm", 16 * 24, blk=16)
        rmag = sb("rmag", 16)
        zpad = sb("zpad", 16 * 128, BF16)
        bpad = sb("bpad", 32 * 128, BF16, blk=128)
        cpad = sb("cpad", 32 * 128, BF16)
        tabc = sb("tabc", 16 * 128)
        tabs = sb("tabs", 16 * 128)
        xt = sb("xt", 1024)
        st8 = sb("st8", 32, blk=1)
        xb = sb("xb", 1024, BF16)
        hT = sb("hT", 1024, BF16)
        pT = sb("pT", 12 * 131, BF16)
        uT = sb("uT", 512, BF16)
        zs = sb("zs", 512, BF16)
        gate = sb("gate", 512, BF16)
        qks = sb("qks", 1024, blk=512)
        vT = sb("vT", 512, BF16)
        sq = sb("sq", 1024, BF16)
        tmpbig = sb("tmpbig", 2048, blk=512)
        rinv = Sub(tmpbig, 0, 1024)
        qkn = sb("qkn", 1024, BF16)
        gts = sb("gts", 64, blk=4)
        gh = Sub(tmpbig, 1024, 512)
        gcrow = sb("gcrow", 512)
        egrow = sb("egrow", 512)
        qgT = sb("qgT", 512, BF16)
        kbg = sb("kbg", 512, BF16)
        kdec = sb("kdec", 512, BF16)
        vb = sb("vb", 512, BF16)
        gdf = sb("gdf", 16 * 128, F32, blk=128)
        gdb = sb("gdb", 16 * 128, BF16, blk=128)
        gd = [dict(
            x=Sub(gdf, (4 * p + 0) * 128, 128), xT=Sub(gdf, (4 * p + 1) * 128, 128), pt=Sub(gdf, (4 * p + 2) * 128, 128),
            usb=Sub(gdf, (4 * p + 3) * 128, 128),
            intra=Sub(gdb, (4 * p + 0) * 128, 128), ttb=Sub(gdb, (4 * p + 1) * 128, 128),
            wtb=Sub(gdb, (4 * p + 2) * 128, 128), vn=Sub(gdb, (4 * p + 3) * 128, 128)) for p in range(4)]
        y_sb = Sub(gdf, 0, 512)
        sg = Sub(gdf, 512, 512)
        xo = Sub(gdf, 1024, 1024)
        gy = Sub(gdb, 0, 512)
        junk = Sub(gdb, 1024, 1024)
        s_f = sb("s_f", 512, blk=128)
        s_b = sb("s_b", 512, BF16, blk=128)
        o_sb = sb("o_sb", 512, blk=128)
        mixT = sb("mixT", 1024, BF16, blk=128)
        tmp = [Sub(tmpbig, 512 * i, 512) for i in range(4)]
        sre_b = sb("sre_b", 512, BF16)
        sim_b = sb("sim_b", 512, BF16)
        odn = sre_b
        s5st = sb("s5st", 32, blk=16)
        xt_ = [xt, Sub(stage[0], 0, 1024)]
        uT_ = [uT, sb("uT2", 512, BF16)]
        zs_ = [zs, sb("zs2", 512, BF16)]
        gate_ = [gate, sb("gate2", 512, BF16)]
        lg_ = [sb("lg0", 8), sb("lg1", 8)]

        B = [ps("B%d" % i, 512, F32) for i in range(7)]
        BT = ps("BT", 1024, BF16)

        names = ["e_pe", "e_act", "e_dve", "e_pool", "e_sp"] + ["dma%d" % i for i in range(24)]
        sems = {n: es.enter_context(nc.semaphore(n)) for n in names}
        block = es.enter_context(nc.Block())
        S = Sched(sems)
        S.tilecount = 0
        pe = lambda fn, r=(), w=(), sig=True: S.op("pe", fn, r, w, sig=sig)
        act = lambda fn, r=(), w=(): S.op("act", fn, r, w)
        dve = lambda fn, r=(), w=(): S.op("dve", fn, r, w)
        pool = lambda fn, r=(), w=(): S.op("pool", fn, r, w)

        def load(buf_regs, out_ap, in_ap, key, reads=()):
            return S.op("sp", dma(out_ap, in_ap), reads, buf_regs, dma_sem=S.dsem(key))

        def tap(name, ap, cols, dt=F32, reads=()):
            d = nc.dram_tensor("dbg_" + name, [128, cols], dt, kind="ExternalOutput").ap()
            dbg_out[name] = S.op("sp", dma(d[:, :], ap), reads, (), dma_sem=S.dsem("dbg_" + name))

        S.group_begin()
        for buf, src in ((identf, identf_d), (tri, tri_d), (blkm, blk_d), (maska, maska_d), (maski, maski_d)):
            load([buf.r()], buf.t[:], src[:, :], "const")
        S.group_end(S.dsem("const"))
        act(af(identb.t[:], identf.t[:], AF.Copy), [identf.r()], [identb.r()])
        dve(mset(onesf.t[:], 1.0), (), [onesf.r()])
        dve(mset(onesb.t[:], 1.0), (), [onesb.r()])

        def sm_(i):
            return sm.t[:, 16 * i:16 * i + 16], sm.r(16 * i, 16 * i + 16)

        def layer_setup(l):
            S.group_begin()
            key = "par"
            for buf, src in ((preg, preg_d), (gpost, postg_d), (convw, convw_d), (alog, alog_d), (dtb, dtb_d),
                             (normg, normg_d), (glub, glub_d), (ssmd, ssmd_d), (are, are_d), (aim, aim_d), (ldt, ldt_d)):
                load([buf.r()], buf.t[:], src[l], key)
            load([tmp[0].r()], tmp[0].t[:, 0:256], bre_d[l], key)
            load([tmp[1].r()], tmp[1].t[:, 0:256], bim_d[l], key)
            load([tmp[2].r()], tmp[2].t[:, 0:256], cre_d[l], key)
            load([tmp[3].r()], tmp[3].t[:, 0:256], cim_d[l], key)
            S.group_end(S.dsem(key))

            n = 0
            for kc in range(8):
                for c0 in range(0, INC, SCH):
                    stg = stage[n % 2]
                    n += 1
                    load([stg.r()], stg.t[:, 0:SCH], w_in_d[l, kc * 128:(kc + 1) * 128, c0:c0 + SCH], stg.name)
                    act(af(win_b.t[:, kc * INC + c0: kc * INC + c0 + SCH], stg.t[:, 0:SCH], AF.Copy, scale=preg.t[:, kc:kc + 1]),
                        [stg.r(), preg.r()], [win_b.r()])
            for kc in range(8):
                stg = stage[n % 2]
                n += 1
                load([stg.r()], stg.t[:, 0:1024], w_out_d[l, kc * 128:(kc + 1) * 128, :], stg.name)
                act(af(wout_b.t[:, kc * 1024:(kc + 1) * 1024], stg.t[:, 0:1024], AF.Copy), [stg.r()], [wout_b.r()])
            for kc in range(4):
                stg = stage[n % 2]
                n += 1
                load([stg.r()], stg.t[:, 0:1024], glu_w_d[l, kc * 128:(kc + 1) * 128, :], stg.name)
                act(af(glu_b16.t[:, kc * 1024:(kc + 1) * 1024], stg.t[:, 0:1024], AF.Copy), [stg.r()], [glu_b16.r()])
            for j in range(12):
                for k in range(4):
                    i = j * 4 + k
                    pool(ts(cdiag.t[:, i * 128:(i + 1) * 128], identf.t[:], convw.t[:, i:i + 1], ALU.mult),
                         [identf.r(), convw.r()], [cdiag.r()])
            act(af(ealog.t[:], alog.t[:], AF.Exp), [alog.r()], [ealog.r()])

            (dt_, dt_r), (t0, t0r), (ang, angr), (c_, c_r), (s_, s_r) = sm_(0), sm_(1), sm_(2), sm_(3), sm_(4)
            (c2, c2r), (s2, s2r), (cs, csr) = sm_(5), sm_(6), sm_(7)
            (lbr, lbrr), (lbi, lbir), (den, denr), (cr, crr), (ci, cir) = sm_(8), sm_(9), sm_(10), sm_(11), sm_(12)
            (u1, u1r), (u2, u2r) = sm_(13), sm_(14)
            act(af(dt_, ldt.t[:], AF.Exp), [ldt.r()], [dt_r])
            dve(tt(t0, are.t[:], dt_, ALU.mult), [are.r(), dt_r], [t0r])
            act(af(rmag.t[:], t0, AF.Exp), [t0r], [rmag.r()])
            dve(tt(ang, aim.t[:], dt_, ALU.mult), [aim.r(), dt_r], [angr])
            act(af(s_, ang, AF.Sin, scale=1.0 / 16), [angr], [s_r])
            act(af(c_, ang, AF.Sin, scale=1.0 / 16, bias=float(np.pi / 2)), [angr], [c_r])
            for _ in range(4):
                dve(tt(c2, c_, c_, ALU.mult), [c_r], [c2r])
                dve(tt(s2, s_, s_, ALU.mult), [s_r], [s2r])
                dve(tt(cs, c_, s_, ALU.mult), [c_r, s_r], [csr])
                dve(tt(c_, c2, s2, ALU.subtract), [c2r, s2r], [c_r])
                dve(ts(s_, cs, 2.0, ALU.mult), [csr], [s_r])
            dve(tt(lbr, rmag.t[:], c_, ALU.mult), [rmag.r(), c_r], [lbrr])
            dve(tt(lbi, rmag.t[:], s_, ALU.mult), [rmag.r(), s_r], [lbir])
            dve(tt(u1, are.t[:], are.t[:], ALU.mult), [are.r()], [u1r])
            dve(tt(u2, aim.t[:], aim.t[:], ALU.mult), [aim.r()], [u2r])
            dve(tt(den, u1, u2, ALU.add), [u1r, u2r], [denr])
            dve(lambda e: e.reciprocal(out=den, in_=den), [denr], [denr])
            dve(ts(u1, lbr, -1.0, ALU.add), [lbrr], [u1r])
            dve(tt(cr, u1, are.t[:], ALU.mult), [u1r, are.r()], [crr])
            dve(tt(u2, lbi, aim.t[:], ALU.mult), [lbir, aim.r()], [u2r])
            dve(tt(cr, cr, u2, ALU.add), [crr, u2r], [crr])
            dve(tt(cr, cr, den, ALU.mult), [crr, denr], [crr])
            dve(tt(ci, lbi, are.t[:], ALU.mult), [lbir, are.r()], [cir])
            dve(tt(u2, u1, aim.t[:], ALU.mult), [u1r, aim.r()], [u2r])
            dve(tt(ci, ci, u2, ALU.subtract), [cir, u2r], [cir])
            dve(tt(ci, ci, den, ALU.mult), [cir, denr], [cir])
            b_re3 = tmp[0].t[:, 0:256].rearrange("p (a h) -> p a h", h=16)
            b_im3 = tmp[1].t[:, 0:256].rearrange("p (a h) -> p a h", h=16)
            w1 = tmp[0].t[:, 256:512].rearrange("p (a h) -> p a h", h=16)
            w2 = tmp[1].t[:, 256:512].rearrange("p (a h) -> p a h", h=16)
            crb = cr.unsqueeze(2).to_broadcast([128, 16, 16])
            cib = ci.unsqueeze(2).to_broadcast([128, 16, 16])
            dve(tt(w1, b_re3, crb, ALU.mult), [tmp[0].r(), crr], [tmp[0].r()])
            dve(tt(w2, b_im3, cib, ALU.mult), [tmp[1].r(), cir], [tmp[1].r()])
            dve(tt(w1, w1, w2, ALU.subtract), [tmp[0].r(), tmp[1].r()], [tmp[0].r()])
            dve(tt(w2, b_im3, crb, ALU.mult), [tmp[1].r(), crr], [tmp[1].r()])
            dve(tt(b_re3, b_re3, cib, ALU.mult), [tmp[0].r(), cir], [tmp[0].r()])
            dve(tt(w2, w2, b_re3, ALU.add), [tmp[0].r(), tmp[1].r()], [tmp[1].r()])
            for comp, src in ((0, tmp[0]), (1, tmp[1])):
                dve(mset(zpad.t[:], 0.0), (), [zpad.r()])
                src3 = src.t[:, 256:512].rearrange("p (a h) -> p a h", h=16)
                z3 = zpad.t[:].rearrange("p (a c) -> p a c", c=128)
                for g2 in range(2):
                    for pm in range(4):
                        c0 = 32 * pm + 16 * g2
                        dve(cp(z3[64 * g2:64 * g2 + 64, pm::4, c0:c0 + 16], src3[64 * g2:64 * g2 + 64, pm::4, :]),
                            [src.r()], [zpad.r()])
                for rnd in range(2):
                    for i in range(8):
                        pair = rnd * 8 + i
                        pe(tr(BT.t[:, i * 128:(i + 1) * 128], zpad.t[:, pair * 128:(pair + 1) * 128], identb.t[:]),
                           [zpad.r(), identb.r()], [BT.r()], sig=(i == 7))
                    for i in range(8):
                        pair = rnd * 8 + i
                        k = pair * 2 + comp
                        act(af(bpad.t[:, k * 128:(k + 1) * 128], BT.t[:, i * 128:(i + 1) * 128], AF.Copy), [BT.r()],
                            [bpad.r(k * 128, (k + 1) * 128)])
            dve(mset(cpad.t[:], 0.0), (), [cpad.r()])
            cp3 = cpad.t[:].rearrange("p (a c) -> p a c", c=128)
            for comp, src in ((0, tmp[2]), (1, tmp[3])):
                src3 = src.t[:, 0:256].rearrange("p (a h) -> p a h", h=16)
                for g2 in range(2):
                    for pm in range(4):
                        c0 = 32 * pm + 16 * g2
                        dst = cp3[64 * g2:64 * g2 + 64, (2 * pm + comp)::8, c0:c0 + 16]
                        s_ap = src3[64 * g2:64 * g2 + 64, pm::4, :]
                        if comp == 0:
                            dve(cp(dst, s_ap), [src.r()], [cpad.r()])
                        else:
                            dve(ts(dst, s_ap, -1.0, ALU.mult), [src.r()], [cpad.r()])
            tc3 = tabc.t[:].rearrange("p (a t) -> p a t", t=128)
            ts3 = tabs.t[:].rearrange("p (a t) -> p a t", t=128)
            w3 = [tmp[i].t[:, 0:1024 // 2].rearrange("p (a t) -> p a t", a=16) for i in range(4)]
            dve(cp(tc3[:, :, 0:1], c_.unsqueeze(2)), [c_r], [tabc.r()])
            dve(cp(ts3[:, :, 0:1], s_.unsqueeze(2)), [s_r], [tabs.r()])
            n_ = 1
            while n_ < 128:
                cn = tc3[:, :, n_ - 1:n_].to_broadcast([128, 16, n_])
                sn = ts3[:, :, n_ - 1:n_].to_broadcast([128, 16, n_])
                if n_ <= 32:
                    a1, a2, a3, a4 = (w3[i][:, :, 0:n_] for i in range(4))
                    regs = [tmp[i].r() for i in range(4)]
                    dve(tt(a1, tc3[:, :, 0:n_], cn, ALU.mult), [tabc.r()], [regs[0]])
                    dve(tt(a2, ts3[:, :, 0:n_], sn, ALU.mult), [tabs.r()], [regs[1]])
                    dve(tt(a3, tc3[:, :, 0:n_], sn, ALU.mult), [tabc.r(), tabs.r()], [regs[2]])
                    dve(tt(a4, ts3[:, :, 0:n_], cn, ALU.mult), [tabc.r(), tabs.r()], [regs[3]])
                    dve(tt(tc3[:, :, n_:2 * n_], a1, a2, ALU.subtract), [regs[0], regs[1]], [tabc.r()])
                    dve(tt(ts3[:, :, n_:2 * n_], a3, a4, ALU.add), [regs[2], regs[3]], [tabs.r()])
                else:
                    for h0 in (0, 32):
                        cnh = tc3[:, :, n_ - 1:n_].to_broadcast([128, 16, 32])
                        snh = ts3[:, :, n_ - 1:n_].to_broadcast([128, 16, 32])
                        a1, a2, a3, a4 = (w3[i][:, :, 0:32] for i in range(4))
                        regs = [tmp[i].r() for i in range(4)]
                        dve(tt(a1, tc3[:, :, h0:h0 + 32], cnh, ALU.mult), [tabc.r()], [regs[0]])
                        dve(tt(a2, ts3[:, :, h0:h0 + 32], snh, ALU.mult), [tabs.r()], [regs[1]])
                        dve(tt(a3, tc3[:, :, h0:h0 + 32], snh, ALU.mult), [tabc.r(), tabs.r()], [regs[2]])
                        dve(tt(a4, ts3[:, :, h0:h0 + 32], cnh, ALU.mult), [tabc.r(), tabs.r()], [regs[3]])
                        dve(tt(tc3[:, :, n_ + h0:n_ + h0 + 32], a1, a2, ALU.subtract), [regs[0], regs[1]], [tabc.r()])
                        dve(tt(ts3[:, :, n_ + h0:n_ + h0 + 32], a3, a4, ALU.add), [regs[2], regs[3]], [tabs.r()])
                n_ *= 2

        ST_SSQ, ST_RSTD, ST_SSQ2A, ST_SSQ2B, ST_RSTD2, ST_OSSQ, ST_ORSTD = 0, 1, 2, 3, 4, 8, 12

        def stc(i, n=1):
            return st8.t[:, i:i + n], st8.r(i, i + n)

        G_BETA, G_X, G_NA, G_E, G_L, G_SP, G_G, G_GC, G_GL, G_BG, G_KD, G_T = range(12)

        def gt(i):
            return gts.t[:, 4 * i:4 * i + 4], gts.r(4 * i, 4 * i + 4)

        def front_a(par, first, src_ap, src_dep):
            xt, uT, zs, gate, lg = xt_[par], uT_[par], zs_[par], gate_[par], lg_[par]
            lab = "FA%d" % S.tilecount
            S.tilecount += 1

            def _lab(g):
                while True:
                    S.label = lab
                    try:
                        next(g)
                    except StopIteration:
                        S.label = None
                        return
                    S.label = None
                    yield
            yield from _lab(front_a_body(par, first, src_ap, src_dep, xt, uT, zs, gate, lg))

        def front_a_body(par, first, src_ap, src_dep, xt, uT, zs, gate, lg):
            load([xt.r()], xt.t[:], src_ap, "xt%d" % par, reads=src_dep)
            (ssq, ssq_r), (rstd, rstd_r) = stc(ST_SSQ), stc(ST_RSTD)
            dve(mset(st8.t[:, 0:2], 0.0), (), [st8.r(0, 2)])
            act(af(xb.t[:], xt.t[:], AF.Square, accum=ssq), [xt.r()], [xb.r(), ssq_r])
            dve(ts(rstd, ssq, 1.0 / D, ALU.mult, EPS, ALU.add), [ssq_r], [rstd_r])
            act(af(rstd, rstd, AF.Sqrt), [rstd_r], [rstd_r])
            dve(lambda e: e.reciprocal(out=rstd, in_=rstd), [rstd_r], [rstd_r])
            act(af(xb.t[:], xt.t[:], AF.Copy, scale=rstd), [xt.r(), rstd_r], [xb.r()])
            yield
            for kc in range(8):
                pe(tr(BT.t[:, kc * 128:(kc + 1) * 128], xb.t[:, kc * 128:(kc + 1) * 128], identb.t[:]), [xb.r(), identb.r()],
                   [BT.r()], sig=(kc == 7))
            act(af(hT.t[:], BT.t[:], AF.Copy), [BT.r()], [hT.r()])
            yield
            bank = B[6]
            pT3 = pT.t[:].rearrange("p (j t) -> p j t", t=131)
            if first:
                pool(mset(pT.t[:], 0.0), (), [pT.r()])

            def win_fm(col0):
                for j in range(4):
                    for kc in range(8):
                        c = kc * INC + col0 + j * 128
                        pe(mm(bank.t[:, j * 128:(j + 1) * 128], win_b.t[:, c:c + 128], hT.t[:, kc * 128:(kc + 1) * 128],
                              start=(kc == 0), stop=(kc == 7)), [win_b.r(), hT.r()], [bank.r()],
                           sig=(j == 3 and kc == 7))
                    yield
            for g3 in range(3):
                yield from win_fm(g3 * 512)
                act(af(pT3[:, 4 * g3:4 * g3 + 4, 3:131], bank.t[:].rearrange("p (j t) -> p j t", t=128), AF.Copy),
                    [bank.r()], [pT.r()])
            yield from win_fm(2056)
            act(af(uT.t[:], bank.t[:], AF.Copy), [bank.r()], [uT.r()])
            yield from win_fm(2568)
            act(af(zs.t[:], bank.t[:], AF.Silu), [bank.r()], [zs.r()])
            for kc in range(8):
                pe(mm(bank.t[:, :], hT.t[:, kc * 128:(kc + 1) * 128], win_b.t[:, kc * INC + 1536: kc * INC + 2048],
                      start=(kc == 0), stop=(kc == 7)), [win_b.r(), hT.r()], [bank.r()], sig=(kc == 7))
                if kc % 4 == 3:
                    yield
            act(af(gate.t[:], bank.t[:], AF.Silu), [bank.r()], [gate.r()])
            for kc in range(8):
                pe(mm(bank.t[:, 0:8], hT.t[:, kc * 128:(kc + 1) * 128], win_b.t[:, kc * INC + 2048: kc * INC + 2056],
                      start=(kc == 0), stop=(kc == 7)), [win_b.r(), hT.r()], [bank.r()], sig=(kc == 7))
            act(af(lg.t[:], bank.t[:, 0:8], AF.Copy), [bank.r()], [lg.r()])
            yield

        CB = [B[2], B[3], B[6]]

        (beta, beta_r), (gx, gx_r), (gna, gna_r), (ge, ge_r), (gl_, gl_r), (gsp, gsp_r), (gg, gg_r) = \
            gt(G_BETA), gt(G_X), gt(G_NA), gt(G_E), gt(G_L), gt(G_SP), gt(G_G)
        (gc, gc_r), (glast, glast_r), (bg, bg_r), (kd, kd_r), (gtmp, gtmp_r) = gt(G_GC), gt(G_GL), gt(G_BG), gt(G_KD), gt(G_T)

        def b3(ap):
            return ap.unsqueeze(2).to_broadcast([128, 4, 128])

        def front_b(par, first, dbg):
            xt, uT, zs, gate, lg = xt_[par], uT_[par], zs_[par], gate_[par], lg_[par]
            pT3 = pT.t[:].rearrange("p (j t) -> p j t", t=131)
            act(af(beta, lg.t[:, 0:4], AF.Sigmoid), [lg.r()], [beta_r])
            dve(tt(gx, lg.t[:, 4:8], dtb.t[:], ALU.add), [lg.r(), dtb.r()], [gx_r])
            dve(stt(gna, gx, -1.0, gx, ALU.mult, ALU.min), [gx_r], [gna_r])
            act(af(ge, gna, AF.Exp), [gna_r], [ge_r])
            act(af(gl_, ge, AF.Ln, bias=1.0), [ge_r], [gl_r])
            dve(stt(gsp, gx, 0.0, gl_, ALU.max, ALU.add), [gx_r, gl_r], [gsp_r])
            dve(stt(gg, gsp, -1.0, ealog.t[:], ALU.mult, ALU.mult), [gsp_r, ealog.r()], [gg_r])
            yield

            for j in range(12):
                bank = CB[j // 4]
                jj = j % 4
                for k in range(4):
                    i = j * 4 + k
                    pe(mm(bank.t[:, jj * 128:(jj + 1) * 128], cdiag.t[:, i * 128:(i + 1) * 128], pT.t[:, j * 131 + k: j * 131 + k + 128],
                          start=(k == 0), stop=(k == 3)), [cdiag.r(), pT.r()], [bank.r()], sig=(jj == 3 and k == 3))
            act(af(qks.t[:, 0:512], CB[0].t[:], AF.Silu), [CB[0].r()], [qks.r()])
            act(af(qks.t[:, 512:1024], CB[1].t[:], AF.Silu), [CB[1].r()], [qks.r()])
            act(af(vT.t[:], CB[2].t[:], AF.Silu), [CB[2].r()], [vT.r()])
            pool(cp(pT3[:, :, 0:3], pT3[:, :, 128:131]), [pT.r()], [pT.r()])
            yield

            pool(tt(sq.t[:], qks.t[:], qks.t[:], ALU.mult), [qks.r()], [sq.r()])
            pe(mm(CB[0].t[:], onesb.t[:], sq.t[:, 0:512]), [onesb.r(), sq.r()], [CB[0].r()])
            pe(mm(CB[1].t[:], onesb.t[:], sq.t[:, 512:1024]), [onesb.r(), sq.r()], [CB[1].r()])
            yield
            act(af(rinv.t[:, 0:512], CB[0].t[:], AF.Sqrt, scale=128.0, bias=128.0 * EPS), [CB[0].r()], [rinv.r()])
            act(af(rinv.t[:, 512:1024], CB[1].t[:], AF.Sqrt, bias=EPS), [CB[1].r()], [rinv.r()])
            dve(lambda e: e.reciprocal(out=rinv.t[:], in_=rinv.t[:]), [rinv.r()], [rinv.r()])
            dve(tt(qkn.t[:], qks.t[:], rinv.t[:], ALU.mult), [qks.r(), rinv.r()], [qkn.r()])
            yield

            pe(mm(CB[2].t[:, 8:12], tri.t[:], gg), [tri.r(), gg_r], [CB[2].r()], sig=False)
            pe(mm(CB[2].t[:, 12:16], blkm.t[:], gg), [blkm.r(), gg_r], [CB[2].r()])
            dve(cp(gts.t[:, 4 * G_GC:4 * G_GC + 8], CB[2].t[:, 8:16]), [CB[2].r()], [gc_r, glast_r])
            yield
            for h in range(NH):
                dve(ts(gh.t[:, h * 128:(h + 1) * 128], tri.t[:], gts.t[:, 4 * G_G + h:4 * G_G + h + 1], ALU.mult),
                    [tri.r(), gg_r], [gh.r()])
            for h in range(NH):
                pe(mm(CB[0].t[:, h * 128:(h + 1) * 128], onesf.t[:], gh.t[:, h * 128:(h + 1) * 128]), [onesf.r(), gh.r()],
                   [CB[0].r()], sig=(h == NH - 1))
            act(af(gcrow.t[:], CB[0].t[:], AF.Copy), [CB[0].r()], [gcrow.r()])
            act(af(egrow.t[:], gcrow.t[:], AF.Exp), [gcrow.r()], [egrow.r()])
            yield
            dve(tt(qgT.t[:], qkn.t[:, 0:512], egrow.t[:], ALU.mult), [qkn.r(), egrow.r()], [qgT.r()])
            act(af(gtmp, gc, AF.Exp), [gc_r], [gtmp_r])
            dve(tt(bg, beta, gtmp, ALU.mult), [beta_r, gtmp_r], [bg_r])
            dve(tt(kd, glast, gc, ALU.subtract), [glast_r, gc_r], [kd_r])
            act(af(kd, kd, AF.Exp), [kd_r], [kd_r])
            yield
            for h in range(NH):
                pe(tr(BT.t[:, h * 128:(h + 1) * 128], qkn.t[:, 512 + h * 128: 512 + (h + 1) * 128], identb.t[:]),
                   [qkn.r(), identb.r()], [BT.r()], sig=False)
            for h in range(NH):
                pe(tr(BT.t[:, 512 + h * 128:512 + (h + 1) * 128], vT.t[:, h * 128:(h + 1) * 128], identb.t[:]),
                   [vT.r(), identb.r()], [BT.r()], sig=(h == NH - 1))
            k3 = BT.t[:, 0:512].rearrange("p (h d) -> p h d", d=128)
            v3 = BT.t[:, 512:1024].rearrange("p (h d) -> p h d", d=128)

            dve(tt(kbg.t[:].rearrange("p (h d) -> p h d", d=128), k3, b3(bg), ALU.mult), [BT.r(), bg_r], [kbg.r()])
            dve(tt(kdec.t[:].rearrange("p (h d) -> p h d", d=128), k3, b3(kd), ALU.mult), [BT.r(), kd_r], [kdec.r()])
            dve(tt(vb.t[:].rearrange("p (h d) -> p h d", d=128), v3, b3(beta), ALU.mult), [BT.r(), beta_r], [vb.r()])
            yield

            if first:
                dve(mset(s_f.t[:], 0.0), (), [s_f.r()])
                dve(mset(s_b.t[:], 0.0), (), [s_b.r()])
                dve(mset(s5st.t[:], 0.0), (), [s5st.r()])

            if "pre" in dbg:
                tap("qkn", qkn.t[:], 1024, BF16, [qkn.r()])
                tap("vT", vT.t[:], 512, BF16, [vT.r()])
                tap("gts", gts.t[:], 64, F32, [gts.r()])
                tap("gcrow", gcrow.t[:], 512, F32, [gcrow.r()])
                tap("uT", uT.t[:], 512, BF16, [uT.r()])
                tap("ealog", ealog.t[:], 4, F32, [ealog.r()])
                tap("alog", alog.t[:], 4, F32, [alog.r()])


        def mixers(par, nxt_gen, curt, dbg):
            xt, uT, zs, gate, lg = xt_[par], uT_[par], zs_[par], gate_[par], lg_[par]
            def gdn_head(h):
                g_ = gd[h]
                Bk = B[h]
                hs = slice(h * 128, (h + 1) * 128)
                kT_h = qkn.t[:, 512 + h * 128: 512 + (h + 1) * 128]
                qT_h = qkn.t[:, hs]
                S0, S1, S2, S3 = (Bk.t[:, i * 128:(i + 1) * 128] for i in range(4))
                X, XT, PT = g_["x"], g_["xT"], g_["pt"]
                bk = [Bk.r()]
                pe(mm(S0, kT_h, kT_h), [qkn.r()], bk, sig=False)
                pe(mm(S1, kT_h, qT_h), [qkn.r()], bk)
                yield
                gcol = gts.t[:, 4 * G_GC + h:4 * G_GC + h + 1]
                dve(stt(X.t[:], gcrow.t[:, hs], gcol, maska.t[:], ALU.subtract, ALU.max), [gcrow.r(), gc_r, maska.r()], [X.r()])
                act(af(X.t[:], X.t[:], AF.Exp, scale=-1.0), [X.r()], [X.r()])
                dve(stt(XT.t[:], gcrow.t[:, hs], gcol, maski.t[:], ALU.subtract, ALU.min), [gcrow.r(), gc_r, maski.r()], [XT.r()])
                act(af(XT.t[:], XT.t[:], AF.Exp), [XT.r()], [XT.r()])
                yield
                dve(stt(X.t[:], S0, gts.t[:, 4 * G_BETA + h:4 * G_BETA + h + 1], X.t[:], ALU.mult, ALU.mult),
                    bk + [beta_r, X.r()], [X.r()])
                dve(tt(g_["intra"].t[:], S1, XT.t[:], ALU.mult), bk + [XT.r()], [g_["intra"].r()])
                yield
                pe(mm(S2, X.t[:], identf.t[:]), [X.r(), identf.r()], bk)
                yield
                dve(cp(XT.t[:], S2), bk, [XT.r()])
                dve(tt(PT.t[:], identf.t[:], S2, ALU.subtract), [identf.r()] + bk, [PT.r()])
                yield
                for lev in range(1, 6):
                    pe(mm(S0, XT.t[:], X.t[:]), [XT.r(), X.r()], bk, sig=(lev == 5))
                    if lev < 5:
                        pe(mm(S1, X.t[:], XT.t[:]), [XT.r(), X.r()], bk)
                    yield
                    dve(cp(X.t[:], S0), bk, [X.r()])
                    if lev < 5:
                        act(af(XT.t[:], S1, AF.Copy), bk, [XT.r()])
                    yield
                    pe(mm(S2, X.t[:], PT.t[:]), [X.r(), PT.r()], bk)
                    yield
                    if lev < 5:
                        dve(tt(PT.t[:], PT.t[:], S2, ALU.add), [PT.r()] + bk, [PT.r()])
                    else:
                        dve(tt(g_["ttb"].t[:], PT.t[:], S2, ALU.add), [PT.r()] + bk, [g_["ttb"].r()])
                    yield
                pe(mm(S0, kbg.t[:, hs], g_["ttb"].t[:]), [kbg.r(), g_["ttb"].r()], bk, sig=False)
                pe(mm(S1, g_["ttb"].t[:], vb.t[:, hs]), [vb.r(), g_["ttb"].r()], bk)
                yield
                act(af(g_["wtb"].t[:], S0, AF.Copy), bk, [g_["wtb"].r()])
                dve(cp(g_["usb"].t[:], S1), bk, [g_["usb"].r()])
                yield
                sbr = s_b.r(h * 128, h * 128 + 128)
                sfr = s_f.r(h * 128, h * 128 + 128)
                for j in range(2):
                    r0, r1 = 64 * j, 64 * j + 64
                    pe(mm(Bk.t[r0:r1, 256:384], g_["wtb"].t[:, r0:r1], s_b.t[:, hs]), [g_["wtb"].r(), sbr], bk)
                    yield
                    dve(tt(g_["vn"].t[r0:r1, :], g_["usb"].t[r0:r1, :], Bk.t[r0:r1, 256:384], ALU.subtract),
                        [g_["usb"].r()] + bk, [g_["vn"].r()])
                    yield
                    pe(mm(Bk.t[r0:r1, 384:512], qgT.t[:, h * 128 + r0:h * 128 + r1], s_b.t[:, hs], start=True, stop=False),
                       [qgT.r(), sbr], bk, sig=False)
                    pe(mm(Bk.t[r0:r1, 384:512], g_["intra"].t[r0:r1, r0:r1], g_["vn"].t[r0:r1, :], start=False, stop=True),
                       [g_["intra"].r(), g_["vn"].r()], bk, sig=False)
                    pe(mm(S2, kdec.t[r0:r1, hs], g_["vn"].t[r0:r1, :]), [kdec.r(), g_["vn"].r()], bk)
                    yield
                    egl = egrow.t[:, h * 128 + r1 - 1:h * 128 + r1]
                    dve(stt(s_f.t[:, hs], s_f.t[:, hs], egl, S2, ALU.mult, ALU.add), [sfr, egrow.r()] + bk, [sfr])
                    act(af(s_b.t[:, hs], s_f.t[:, hs], AF.Copy), [sfr], [sbr])
                    yield
                act(af(o_sb.t[:, hs], S3, AF.Copy), bk, [o_sb.r(h * 128, h * 128 + 128)])

            Y = B[5]

            def s5_ct(ct):
                V = B[4]
                cosT = tabc.t[:, ct * 512:(ct + 1) * 512]
                sinT = tabs.t[:, ct * 512:(ct + 1) * 512]
                t1, t2, t3, t4 = tmp
                for pm in range(4):
                    pair = 4 * ct + pm
                    pe(mm(V.t[:, pm * 128:(pm + 1) * 128], bpad.t[:, (2 * pair) * 128:(2 * pair + 1) * 128], uT.t[:, ct * 128:(ct + 1) * 128]),
                       [bpad.r(), uT.r()], [V.r()], sig=(pm == 3))
                yield
                dve(tt(t1.t[:], V.t[:], cosT, ALU.mult), [V.r(), tabc.r()], [t1.r()])
                dve(tt(t2.t[:], V.t[:], sinT, ALU.mult), [V.r(), tabs.r()], [t2.r()])
                yield
                for pm in range(4):
                    pair = 4 * ct + pm
                    pe(mm(V.t[:, pm * 128:(pm + 1) * 128], bpad.t[:, (2 * pair + 1) * 128:(2 * pair + 2) * 128], uT.t[:, ct * 128:(ct + 1) * 128]),
                       [bpad.r(), uT.r()], [V.r()], sig=(pm == 3))
                yield
                dve(tt(t3.t[:], V.t[:], sinT, ALU.mult), [V.r(), tabs.r()], [t3.r()])
                dve(tt(t4.t[:], V.t[:], cosT, ALU.mult), [V.r(), tabc.r()], [t4.r()])
                yield
                dve(tt(t1.t[:], t1.t[:], t3.t[:], ALU.add), [t1.r(), t3.r()], [t1.r()])
                dve(tt(t2.t[:], t4.t[:], t2.t[:], ALU.subtract), [t2.r(), t4.r()], [t2.r()])
                yield
                for pm in range(4):
                    pair = 4 * ct + pm
                    rb = rmag.t[:, pair:pair + 1].to_broadcast([128, 128])
                    dve(scan(t3.t[:, pm * 128:(pm + 1) * 128], rb, t1.t[:, pm * 128:(pm + 1) * 128], s5st.t[:, pair:pair + 1]),
                        [rmag.r(), t1.r(), s5st.r()], [t3.r()])
                    yield
                for pm in range(4):
                    pair = 4 * ct + pm
                    rb = rmag.t[:, pair:pair + 1].to_broadcast([128, 128])
                    dve(scan(t4.t[:, pm * 128:(pm + 1) * 128], rb, t2.t[:, pm * 128:(pm + 1) * 128], s5st.t[:, 16 + pair:17 + pair]),
                        [rmag.r(), t2.r(), s5st.r()], [t4.r()])
                    yield
                dve(tt(t1.t[:], t3.t[:], cosT, ALU.mult), [t3.r(), tabc.r()], [t1.r()])
                dve(tt(t2.t[:], t4.t[:], sinT, ALU.mult), [t4.r(), tabs.r()], [t2.r()])
                dve(tt(sre_b.t[:], t1.t[:], t2.t[:], ALU.subtract), [t1.r(), t2.r()], [sre_b.r()])
                yield
                l1 = t1.t[:].rearrange("p (a t) -> p a t", t=128)[:, :, 127:128]
                l2 = t2.t[:].rearrange("p (a t) -> p a t", t=128)[:, :, 127:128]
                dve(tt(s5st.t[:, 4 * ct:4 * ct + 4].unsqueeze(2), l1, l2, ALU.subtract), [t1.r(), t2.r()], [s5st.r(0, 16)])
                yield
                dve(tt(t1.t[:], t3.t[:], sinT, ALU.mult), [t3.r(), tabs.r()], [t1.r()])
                dve(tt(t2.t[:], t4.t[:], cosT, ALU.mult), [t4.r(), tabc.r()], [t2.r()])
                dve(tt(sim_b.t[:], t1.t[:], t2.t[:], ALU.add), [t1.r(), t2.r()], [sim_b.r()])
                yield
                dve(tt(s5st.t[:, 16 + 4 * ct:16 + 4 * ct + 4].unsqueeze(2), l1, l2, ALU.add), [t1.r(), t2.r()], [s5st.r(16, 32)])
                yield
                for pm in range(4):
                    pair = 4 * ct + pm
                    pe(mm(Y.t[:, ct * 128:(ct + 1) * 128], cpad.t[:, (2 * pair) * 128:(2 * pair + 1) * 128], sre_b.t[:, pm * 128:(pm + 1) * 128],
                          start=(pm == 0), stop=False), [cpad.r(), sre_b.r()], [Y.r()], sig=False)
                    pe(mm(Y.t[:, ct * 128:(ct + 1) * 128], cpad.t[:, (2 * pair + 1) * 128:(2 * pair + 2) * 128], sim_b.t[:, pm * 128:(pm + 1) * 128],
                          start=False, stop=(pm == 3)), [cpad.r(), sim_b.r()], [Y.r()], sig=(pm == 3))

            def chain(*gens):
                for g in gens:
                    yield from g

            def run_rr(gens):
                gens = list(gens)
                while gens:
                    for g in list(gens):
                        try:
                            next(g)
                        except StopIteration:
                            gens.remove(g)

            def onorm():
                (ossq, ossq_r), (orstd, orstd_r) = stc(ST_OSSQ, 4), stc(ST_ORSTD, 4)
                for h in range(NH):
                    act(af(junk.t[:, 0:128], o_sb.t[:, h * 128:(h + 1) * 128], AF.Square, accum=st8.t[:, ST_OSSQ + h:ST_OSSQ + h + 1]),
                        [o_sb.r()], [junk.r(), ossq_r])
                dve(ts(orstd, ossq, 1.0 / 128, ALU.mult, EPS, ALU.add), [ossq_r], [orstd_r])
                act(af(orstd, orstd, AF.Sqrt), [orstd_r], [orstd_r])
                dve(lambda e: e.reciprocal(out=orstd, in_=orstd), [orstd_r], [orstd_r])
                yield
                o3 = o_sb.t[:].rearrange("p (h d) -> p h d", d=128)
                dve(tt(o3, o3, b3(orstd), ALU.mult), [o_sb.r(), orstd_r], [o_sb.r()])
                dve(tt(o3, o3, normg.t[:].unsqueeze(1).to_broadcast([128, 4, 128]), ALU.mult), [o_sb.r(), normg.r()], [o_sb.r()])
                dve(tt(odn.t[:], o_sb.t[:], gate.t[:], ALU.mult), [o_sb.r(), gate.r()], [odn.r()])
                yield
                for h in range(NH):
                    pe(tr(BT.t[:, h * 128:(h + 1) * 128], odn.t[:, h * 128:(h + 1) * 128], identb.t[:]), [odn.r(), identb.r()], [BT.r()],
                       sig=(h == NH - 1))
                act(af(mixT.t[:, 0:512], BT.t[:, 0:512], AF.Copy), [BT.r()], [mixT.r(0, 512)])
                yield
                if "gdn" in dbg:
                    tap("odn", odn.t[:], 512, BF16, [odn.r()])
                    tap("osb", o_sb.t[:], 512, F32, [o_sb.r()])
                yield

            def run_phase(heads, s5chain):
                heads = list(heads)
                labels = {id(g): "GDN%d" % curt for g in heads}
                labels[id(s5chain)] = "S5_%d" % curt
                tail = onorm()
                labels[id(tail)] = "ON%d" % curt
                live = {"s5": s5chain, "nx": nxt_gen, "tail": None}

                def step(g):
                    S.label = labels.get(id(g))
                    try:
                        next(g)
                        return True
                    except StopIteration:
                        return False
                    finally:
                        S.label = None
                while heads or live["s5"] is not None or live["tail"] is not None:
                    for g in list(heads):
                        if not step(g):
                            heads.remove(g)
                    if not heads and live["tail"] is None and tail is not None:
                        live["tail"] = tail
                        tail = None
                    elif live["tail"] is not None:
                        if not step(live["tail"]):
                            live["tail"] = None
                    for _ in range(S5_STEPS):
                        if live["s5"] is not None and not step(live["s5"]):
                            live["s5"] = None
                    if live["nx"] is not None and not step(live["nx"]):
                        live["nx"] = None

            run_phase([gdn_head(0), gdn_head(1), gdn_head(2), gdn_head(3)], chain(s5_ct(0), s5_ct(1), s5_ct(2), s5_ct(3)))
            if nxt_gen is not None:
                for _ in nxt_gen:
                    pass

        def epilogue(par, dst_ap, dst_regs, dbg, res):
            xt, uT, zs, gate, lg = xt_[par], uT_[par], zs_[par], gate_[par], lg_[par]
            Y = B[5]
            dve(mset(st8.t[:, 2:16], 0.0), (), [st8.r(2, 16)])
            for ct in range(4):
                cs_ = slice(ct * 128, (ct + 1) * 128)
                dve(stt(y_sb.t[:, cs_], uT.t[:, cs_], ssmd.t[:, ct:ct + 1], Y.t[:, cs_], ALU.mult, ALU.add), [uT.r(), ssmd.r(), Y.r()],
                    [y_sb.r()])
            act(af(gy.t[:], y_sb.t[:], AF.Gelu_apprx_tanh if GELU_TANH else AF.Gelu), [y_sb.r()], [gy.r()])
            yield
            if "s5" in dbg:
                tap("ysb", y_sb.t[:], 512, F32, [y_sb.r()])
            for half, bank in ((0, B[4]), (1, B[5])):
                for j in range(4):
                    for kc in range(4):
                        c = kc * 1024 + half * 512 + j * 128
                        pe(mm(bank.t[:, j * 128:(j + 1) * 128], glu_b16.t[:, c:c + 128], gy.t[:, kc * 128:(kc + 1) * 128],
                              start=(kc == 0), stop=(kc == 3)), [glu_b16.r(), gy.r()], [bank.r()], sig=(j == 3 and kc == 3))
                    yield
            for j in range(4):
                act(af(sg.t[:, j * 128:(j + 1) * 128], B[5].t[:, j * 128:(j + 1) * 128], AF.Sigmoid, bias=glub.t[:, 4 + j:5 + j]),
                    [B[5].r(), glub.r()], [sg.r()])
            dve(tt(sg.t[:], sg.t[:], zs.t[:], ALU.mult), [sg.r(), zs.r()], [sg.r()])
            yield
            for j in range(4):
                dve(stt(mixT.t[:, 512 + j * 128:512 + (j + 1) * 128], B[4].t[:, j * 128:(j + 1) * 128], glub.t[:, j:j + 1],
                        sg.t[:, j * 128:(j + 1) * 128], ALU.add, ALU.mult), [B[4].r(), glub.r(), sg.r()], [mixT.r(512, 1024)])

            for half in range(2):
                for kc in range(8):
                    pe(mm(B[half].t[:], mixT.t[:, kc * 128:(kc + 1) * 128], wout_b.t[:, kc * 1024 + half * 512: kc * 1024 + half * 512 + 512],
                          start=(kc == 0), stop=(kc == 7)), [mixT.r(), wout_b.r()], [B[half].r()], sig=(kc == 7))
                    if kc % 4 == 3:
                        yield
            (sa, sa_r), (sb_, sb_r), (r2, r2_r) = stc(ST_SSQ2A), stc(ST_SSQ2B), stc(ST_RSTD2)
            for half in range(2 if NEW_TAIL else 0):
                hs_ = slice(half * 512, (half + 1) * 512)
                dve(tt(xo.t[:, hs_], B[half].t[:], gpost.t[:, hs_], ALU.mult), [B[half].r(), gpost.r()], [xo.r()])
            act(af(junk.t[:, 0:512], B[0].t[:], AF.Square, accum=sa), [B[0].r()], [junk.r(), sa_r])
            act(af(junk.t[:, 512:1024], B[1].t[:], AF.Square, accum=sb_), [B[1].r()], [junk.r(), sb_r])
            dve(tt(r2, sa, sb_, ALU.add), [sa_r, sb_r], [r2_r])
            dve(ts(r2, r2, 1.0 / D, ALU.mult, EPS, ALU.add), [r2_r], [r2_r])
            act(af(r2, r2, AF.Sqrt), [r2_r], [r2_r])
            dve(lambda e: e.reciprocal(out=r2, in_=r2), [r2_r], [r2_r])
            yield
            if NEW_TAIL:
                dve(stt(xo.t[:], xo.t[:], r2, xt.t[:], ALU.mult, ALU.add), [xo.r(), r2_r, xt.r()], [xo.r()])
            else:
                for half in range(2):
                    hs_ = slice(half * 512, (half + 1) * 512)
                    dve(stt(xo.t[:, hs_], B[half].t[:], r2, gpost.t[:, hs_], ALU.mult, ALU.mult), [B[half].r(), r2_r, gpost.r()], [xo.r()])
                pool(tt(xo.t[:], xo.t[:], xt.t[:], ALU.add), [xo.r(), xt.r()], [xo.r()])
            res["tok"] = S.op("pool", dma(dst_ap, xo.t[:]), [xo.r()], dst_regs, dma_sem=S.dsem("xo"))

            yield

        def rr(gens, labels, reps=None):
            gens = [g for g in gens if g is not None]
            reps = reps or {}
            while gens:
                for g in list(gens):
                    S.label = labels.get(id(g))
                    for _ in range(reps.get(id(g), 1)):
                        try:
                            next(g)
                        except StopIteration:
                            gens.remove(g)
                            break
            S.label = None

        last = None
        for l in range(nlayers):
            layer_setup(l)
            src = x_d if l == 0 else x1_t
            dst = out_d if l == nlayers - 1 else x1_t
            tiles = [(s, it) for s in range(nseq) for it in range(ntile)]

            def mk_front(idx):
                s, it = tiles[idx]
                ti = s * ntile + it
                rows = slice(ti * T, (ti + 1) * T)
                src_dep = [(x1_b, ti, ti + 1)] if l > 0 else []
                return front_a(idx % 2, it == 0, src[rows, :], src_dep)

            def mk_fb(idx):
                s, it = tiles[idx]
                dbg = {k for k, v in debug.items() if v == (l, s, it)}
                return front_b(idx % 2, it == 0, dbg)
            for _ in mk_front(0):
                pass
            g0 = mk_fb(0)
            rr([g0], {id(g0): "FB0"})
            for idx, (s, it) in enumerate(tiles):
                ti = s * ntile + it
                rows = slice(ti * T, (ti + 1) * T)
                dst_regs = [(x1_b, ti, ti + 1)] if l < nlayers - 1 else []
                nxt = mk_front(idx + 1) if idx + 1 < len(tiles) else None
                dbg = {k for k, v in debug.items() if v == (l, s, it)}
                mixers(idx % 2, nxt, idx, dbg)
                res = {}
                ge_ = epilogue(idx % 2, dst[rows, :], dst_regs, dbg, res)
                gf_ = mk_fb(idx + 1) if idx + 1 < len(tiles) else None
                rr([ge_, gf_], {id(ge_): "EP%d" % idx, **({id(gf_): "FB%d" % (idx + 1)} if gf_ is not None else {})}, {id(ge_): EP_STEPS})
                last = res["tok"]
        fin = [last] + list(dbg_out.values())
        S.final_wait("pool", fin)
        if VERBOSE:
            print("instr counts", {e: (len(S.streams[e]), sum(len(w) for w, _, _, _, _ in S.streams[e])) for e in S.ENG}, flush=True)
        S.emit(block)
    return nc


GELU_TANH = True
NEW_TAIL = True
S5_STEPS = 2
EP_STEPS = 2
ANNOTATE = False
VERBOSE = False


def prep_params(p, nlayers):
    f = lambda a: np.ascontiguousarray(np.asarray(a, dtype=np.float32))
    L = nlayers
    out = {}
    out["w_in"] = f(p["w_in"][:L])
    out["w_out"] = f(p["w_out"][:L])
    out["glu_w"] = f(p["glu_w"][:L])
    out["preg"] = f(np.asarray(p["pre_norm_g"])[:L].reshape(L, 8, 128).transpose(0, 2, 1))
    out["postg"] = f(np.broadcast_to(np.asarray(p["post_norm_g"])[:L, None, :], (L, 128, 1024)))
    out["convw"] = f(np.asarray(p["conv_w"])[:L].reshape(L, 4, 12, 128).transpose(0, 3, 2, 1).reshape(L, 128, 48))
    out["alog"] = f(np.broadcast_to(np.asarray(p["dn_a_log"])[:L, None, :], (L, 128, 4)))
    out["dtb"] = f(np.broadcast_to(np.asarray(p["dn_dt_bias"])[:L, None, :], (L, 128, 4)))
    out["normg"] = f(np.broadcast_to(np.asarray(p["dn_norm_g"])[:L, None, :], (L, 128, 128)))
    out["glub"] = f(np.asarray(p["glu_b"])[:L].reshape(L, 8, 128).transpose(0, 2, 1))
    out["ssmd"] = f(np.asarray(p["ssm_d"])[:L].reshape(L, 4, 128).transpose(0, 2, 1))

    def gp(a):
        return f(np.asarray(a)[:L].reshape(L, 16, 2, 64).transpose(0, 2, 3, 1).reshape(L, 128, 16))
    out["are"] = gp(p["ssm_a_re"])
    out["aim"] = gp(p["ssm_a_im"])
    out["ldt"] = gp(np.broadcast_to(np.asarray(p["ssm_log_dt"])[:L, :, None], (L, 32, 64)))

    def gb(a):
        return f(np.asarray(a)[:L].reshape(L, 16, 2, 64, 16).transpose(0, 2, 3, 1, 4).reshape(L, 128, 256))
    out["bre"] = gb(p["ssm_b_re"])
    out["bim"] = gb(p["ssm_b_im"])
    out["cre"] = gb(np.asarray(p["ssm_c_re"]).transpose(0, 1, 3, 2))
    out["cim"] = gb(np.asarray(p["ssm_c_im"]).transpose(0, 1, 3, 2))
    i = np.arange(128)
    same = (i[:, None] // 64) == (i[None, :] // 64)
    out["identf"] = np.eye(128, dtype=np.float32)
    out["tri"] = ((i[:, None] <= i[None, :]) & same).astype(np.float32)
    out["blkm"] = same.astype(np.float32)
    out["maska"] = np.where((i[None, :] < i[:, None]) & same, 0.0, 1e4).astype(np.float32)
    out["maski"] = np.where((i[:, None] <= i[None, :]) & same, 0.0, -1e4).astype(np.float32)
    return out


_CACHE = {}


def kernel(**inputs):
    x = np.asarray(inputs["x"], dtype=np.float32)
    bsz, slen, d = x.shape
    nlayers = np.asarray(inputs["w_in"]).shape[0]
    nseq = bsz // NCORES
    key = (nseq, slen, nlayers)
    if key not in _CACHE:
        _CACHE[key] = build_program(nseq, slen, nlayers)
    nc = _CACHE[key]
    params = prep_params(inputs, nlayers)
    in_maps = []
    for c in range(NCORES):
        m = dict(params)
        m["x"] = np.ascontiguousarray(x[c * nseq:(c + 1) * nseq].reshape(nseq * slen, d))
        in_maps.append(m)
    res = run_bass_kernel_spmd(nc, in_maps, core_ids=list(range(NCORES)))
    outs = [np.asarray(r["out"]).reshape(nseq, slen, d) for r in res.results]
    return np.concatenate(outs, axis=0).astype(np.float32)
```
